# Optimizing a Trainium2 kernel written in Bass

```python
import math
import jax, jax.numpy as jnp
from jax import lax
import numpy as np

D_MODEL = 1024
BATCH = 4
SEQ = 8192
DEPTH = 1

NSA_HEADS = 8
NSA_KV_GROUPS = 2
NSA_HEAD_DIM = 64
NSA_REP = NSA_HEADS // NSA_KV_GROUPS
N_BRANCH = 3
CMP_BLOCK = 32
CMP_STRIDE = 16
CMP_HIDDEN = 256
SLC_BLOCK = 64
SLC_TOPK = 16
WINDOW = 512
Q_BLOCK = 128
RET_HEADS = 4
RET_HEAD_DIM = 128
RET_CHUNK = 128
ROPE_BASE = 10000.0
NSA_WIDTH = NSA_HEADS * NSA_HEAD_DIM
NSA_KV_WIDTH = NSA_KV_GROUPS * NSA_HEAD_DIM
RET_WIDTH = RET_HEADS * RET_HEAD_DIM
MIX_WIDTH = NSA_WIDTH + RET_WIDTH
SPLIT_SIZES = (NSA_WIDTH, 2 * N_BRANCH * NSA_KV_WIDTH, NSA_HEADS * N_BRANCH, 3 * RET_WIDTH, RET_WIDTH)
IN_WIDTH = NSA_WIDTH + 2 * N_BRANCH * NSA_KV_WIDTH + NSA_HEADS * N_BRANCH + 3 * RET_WIDTH + RET_WIDTH
D_FF = 2816
PLE_DIM = 256
N_LN = 4
ALPHA = (2.0 * DEPTH) ** 0.25
BETA = (8.0 * DEPTH) ** -0.25
LN_EPS = 1e-5
NEG = -1e30

kernel_name = "hybrid_nsa_retention_macaron_deepnorm"


def _layer_norm(x, g, b):
    xf = x.astype(jnp.float32)
    mu = jnp.mean(xf, axis=-1, keepdims=True)
    var = jnp.mean(jnp.square(xf - mu), axis=-1, keepdims=True)
    y = (xf - mu) * lax.rsqrt(var + LN_EPS)
    return (y * g.astype(jnp.float32) + b.astype(jnp.float32)).astype(x.dtype)


def _swiglu(x, w13, w2):
    a, u = jnp.split(x @ w13, 2, axis=-1)
    return (jax.nn.silu(a) * u) @ w2


def _rotary(x, pos):
    d = x.shape[-1]
    freqs = ROPE_BASE ** (-jnp.arange(0, d, 2, dtype=jnp.float32) / d)
    ang = pos[:, None] * freqs[None, :]
    cos = jnp.cos(ang)[None, :, None, :]
    sin = jnp.sin(ang)[None, :, None, :]
    x1, x2 = jnp.split(x, 2, axis=-1)
    out = jnp.concatenate([x1 * cos - x2 * sin, x1 * sin + x2 * cos], axis=-1)
    return out.astype(x.dtype)


def _compress(kraw, pos_emb, w1, w2):
    B, G, T, dh = kraw.shape
    n_cmp = (T - CMP_BLOCK) // CMP_STRIDE + 1
    idx = jnp.arange(n_cmp)[:, None] * CMP_STRIDE + jnp.arange(CMP_BLOCK)[None, :]
    blocks = kraw[:, :, idx, :] + pos_emb
    flat = blocks.reshape(B, G, n_cmp, CMP_BLOCK * dh)
    return jax.nn.gelu(flat @ w1) @ w2


def _nsa(q, gates, k_cmp, v_cmp, k_slc, v_slc, k_win, v_win):
    B, G, R, T, dh = q.shape
    n_cmp = k_cmp.shape[2]
    n_slc = T // SLC_BLOCK
    topk = min(SLC_TOPK, n_slc)
    scale = dh ** -0.5
    cmp_start = jnp.arange(n_cmp) * CMP_STRIDE
    cmp_last = cmp_start + CMP_BLOCK - 1
    slc_start = jnp.arange(n_slc) * SLC_BLOCK
    overlap = jnp.clip(jnp.minimum(cmp_start[:, None] + CMP_BLOCK, slc_start[None, :] + SLC_BLOCK)
                       - jnp.maximum(cmp_start[:, None], slc_start[None, :]), 0).astype(jnp.float32) / CMP_BLOCK
    k_blk = k_slc.reshape(B, G, n_slc, SLC_BLOCK, dh)
    v_blk = v_slc.reshape(B, G, n_slc, SLC_BLOCK, dh)
    pad = ((0, 0), (0, 0), (WINDOW, 0), (0, 0))
    k_pad = jnp.pad(k_win, pad)
    v_pad = jnp.pad(v_win, pad)
    bi = jnp.arange(B)[:, None, None, None]
    gi = jnp.arange(G)[None, :, None, None]
    blk_ids = jnp.arange(n_slc)

    def block_fn(qb):
        q0 = qb * Q_BLOCK
        qq = lax.dynamic_slice_in_dim(q, q0, Q_BLOCK, axis=3)
        gg = lax.dynamic_slice_in_dim(gates, q0, Q_BLOCK, axis=3)
        t = q0 + jnp.arange(Q_BLOCK)
        s_c = jnp.einsum('bgrqd,bgcd->bgrqc', qq, k_cmp).astype(jnp.float32) * scale
        mask_c = cmp_last[None, :] <= t[:, None]
        p_c = jax.nn.softmax(jnp.where(mask_c, s_c, NEG), axis=-1) * mask_c
        o_c = jnp.einsum('bgrqc,bgcd->bgrqd', p_c.astype(v_cmp.dtype), v_cmp)
        imp = jnp.einsum('bgrqc,cs->bgqs', p_c, overlap)
        cur = t // SLC_BLOCK
        forced = (blk_ids[None, :] == 0) | (blk_ids[None, :] == cur[:, None]) | (blk_ids[None, :] == cur[:, None] - 1)
        valid_b = slc_start[None, :] <= t[:, None]
        imp = jnp.where(valid_b, jnp.where(forced, jnp.inf, imp), -jnp.inf)
        _, sel = lax.top_k(imp, topk)
        key_pos = sel[..., None] * SLC_BLOCK + jnp.arange(SLC_BLOCK)
        mask_s = (key_pos <= t[None, None, :, None, None]).reshape(B, G, Q_BLOCK, topk * SLC_BLOCK)
        ks = k_blk[bi, gi, sel].reshape(B, G, Q_BLOCK, topk * SLC_BLOCK, dh)
        vs = v_blk[bi, gi, sel].reshape(B, G, Q_BLOCK, topk * SLC_BLOCK, dh)
        s_s = jnp.einsum('bgrqd,bgqkd->bgrqk', qq, ks).astype(jnp.float32) * scale
        p_s = jax.nn.softmax(jnp.where(mask_s[:, :, None], s_s, NEG), axis=-1)
        o_s = jnp.einsum('bgrqk,bgqkd->bgrqd', p_s.astype(vs.dtype), vs)
        kw = lax.dynamic_slice_in_dim(k_pad, q0, WINDOW + Q_BLOCK, axis=2)
        vw = lax.dynamic_slice_in_dim(v_pad, q0, WINDOW + Q_BLOCK, axis=2)
        kpos = q0 - WINDOW + jnp.arange(WINDOW + Q_BLOCK)
        dist = t[:, None] - kpos[None, :]
        mask_w = (kpos[None, :] >= 0) & (dist >= 0) & (dist < WINDOW)
        s_w = jnp.einsum('bgrqd,bgkd->bgrqk', qq, kw).astype(jnp.float32) * scale
        p_w = jax.nn.softmax(jnp.where(mask_w, s_w, NEG), axis=-1)
        o_w = jnp.einsum('bgrqk,bgkd->bgrqd', p_w.astype(vw.dtype), vw)
        out = gg[..., 0:1] * o_c + gg[..., 1:2] * o_s + gg[..., 2:3] * o_w
        return out.astype(q.dtype)

    outs = lax.map(block_fn, jnp.arange(T // Q_BLOCK))
    return outs.transpose(1, 0, 4, 2, 3, 5).reshape(B, T, G * R * dh)


def _retention(q, k, v, gn_g, gn_b):
    B, T, H, d = q.shape
    C = RET_CHUNK
    N = T // C
    gamma = 1.0 - jnp.exp2(-5.0 - jnp.arange(H, dtype=jnp.float32))
    log_g = jnp.log(gamma)
    pos = jnp.arange(T, dtype=jnp.float32)
    q = _rotary(q, pos)
    k = _rotary(k, pos) * (d ** -0.5)
    to_chunks = lambda a: a.reshape(B, N, C, H, d).transpose(0, 3, 1, 2, 4)
    qc, kc, vc = to_chunks(q), to_chunks(k), to_chunks(v)
    i = jnp.arange(C, dtype=jnp.float32)
    rel = i[:, None] - i[None, :]
    decay = jnp.where(rel[None] >= 0, jnp.exp(jnp.maximum(rel, 0.0)[None] * log_g[:, None, None]), 0.0)
    inner = jnp.einsum('bhnid,bhnjd->bhnij', qc, kc) * decay[:, None]
    o_inner = jnp.einsum('bhnij,bhnjd->bhnid', inner, vc)
    zeta = jnp.exp((C - 1.0 - i)[None, :] * log_g[:, None])
    xi = jnp.exp((i + 1.0)[None, :] * log_g[:, None])
    kv = jnp.einsum('bhnjd,bhnje->nbhde', kc * zeta[:, None, :, None], vc)
    g_chunk = jnp.exp(C * log_g)[None, :, None, None]

    def step(state, kv_n):
        return g_chunk * state + kv_n, state

    _, s_prev = lax.scan(step, jnp.zeros((B, H, d, d), kv.dtype), kv)
    o_cross = jnp.einsum('bhnid,nbhde->bhnie', qc * xi[:, None, :, None], s_prev)
    o = (o_inner + o_cross).astype(jnp.float32).transpose(0, 2, 3, 1, 4).reshape(B, T, H, d)
    mu = jnp.mean(o, axis=-1, keepdims=True)
    var = jnp.mean(jnp.square(o - mu), axis=-1, keepdims=True)
    o = ((o - mu) * lax.rsqrt(var + LN_EPS)).reshape(B, T, H * d)
    return (o * gn_g.astype(jnp.float32) + gn_b.astype(jnp.float32)).astype(v.dtype)


def _token_mixer(h, w_in, cmp_pos, cmp_w1, cmp_w2, gn_g, gn_b, w_out):
    B, T, _ = h.shape
    proj = h @ w_in
    cuts = np.cumsum(SPLIT_SIZES)[:-1].tolist()
    q_n, kv_n, g_n, qkv_r, g_r = jnp.split(proj, cuts, axis=-1)
    q_nsa = q_n.reshape(B, T, NSA_KV_GROUPS, NSA_REP, NSA_HEAD_DIM).transpose(0, 2, 3, 1, 4)
    kvs = kv_n.reshape(B, T, 2 * N_BRANCH, NSA_KV_GROUPS, NSA_HEAD_DIM).transpose(2, 0, 3, 1, 4)
    k_cmp = _compress(kvs[0], cmp_pos[0], cmp_w1[0], cmp_w2[0])
    v_cmp = _compress(kvs[1], cmp_pos[1], cmp_w1[1], cmp_w2[1])
    gates = jax.nn.sigmoid(g_n.astype(jnp.float32)).reshape(B, T, NSA_KV_GROUPS, NSA_REP, N_BRANCH).transpose(0, 2, 3, 1, 4)
    o_nsa = _nsa(q_nsa, gates, k_cmp, v_cmp, kvs[2], kvs[3], kvs[4], kvs[5])
    q_r, k_r, v_r = [a.reshape(B, T, RET_HEADS, RET_HEAD_DIM) for a in jnp.split(qkv_r, 3, axis=-1)]
    o_ret = jax.nn.silu(g_r) * _retention(q_r, k_r, v_r, gn_g, gn_b)
    return jnp.concatenate([o_nsa, o_ret.astype(o_nsa.dtype)], axis=-1) @ w_out


def setup_inputs(seed: int = 0) -> dict:
    key = jax.random.key(seed)
    ks = jax.random.split(key, 16)
    f32 = jnp.float32
    nrm = lambda k, shape, s: jax.random.normal(k, shape, f32) * s
    return {
        "x": nrm(ks[0], (BATCH, SEQ, D_MODEL), 1.0),
        "p": nrm(ks[1], (DEPTH, BATCH, SEQ, PLE_DIM), 1.0),
        "ffn_w13": nrm(ks[2], (DEPTH, 2, D_MODEL, 2 * D_FF), D_MODEL ** -0.5),
        "ffn_w2": nrm(ks[3], (DEPTH, 2, D_FF, D_MODEL), BETA * D_FF ** -0.5),
        "w_in": nrm(ks[4], (DEPTH, D_MODEL, IN_WIDTH), D_MODEL ** -0.5),
        "cmp_pos": nrm(ks[5], (DEPTH, 2, CMP_BLOCK, NSA_HEAD_DIM), 0.1),
        "cmp_w1": nrm(ks[6], (DEPTH, 2, CMP_BLOCK * NSA_HEAD_DIM, CMP_HIDDEN), (CMP_BLOCK * NSA_HEAD_DIM) ** -0.5),
        "cmp_w2": nrm(ks[7], (DEPTH, 2, CMP_HIDDEN, NSA_HEAD_DIM), CMP_HIDDEN ** -0.5),
        "ret_gn_g": 1.0 + nrm(ks[8], (DEPTH, RET_WIDTH), 0.02),
        "ret_gn_b": nrm(ks[9], (DEPTH, RET_WIDTH), 0.02),
        "w_out": nrm(ks[10], (DEPTH, MIX_WIDTH, D_MODEL), BETA * MIX_WIDTH ** -0.5),
        "w_ple": nrm(ks[11], (DEPTH, PLE_DIM, D_MODEL), BETA * PLE_DIM ** -0.5),
        "w_ple_gate": nrm(ks[12], (DEPTH, D_MODEL, D_MODEL), D_MODEL ** -0.5),
        "ln_g": 1.0 + nrm(ks[13], (DEPTH, N_LN, D_MODEL), 0.02),
        "ln_b": nrm(ks[14], (DEPTH, N_LN, D_MODEL), 0.02),
    }


def reference(x, p, ffn_w13, ffn_w2, w_in, cmp_pos, cmp_w1, cmp_w2, ret_gn_g, ret_gn_b,
              w_out, w_ple, w_ple_gate, ln_g, ln_b):
    for i in range(DEPTH):
        x = _layer_norm(ALPHA * x + 0.5 * _swiglu(x, ffn_w13[i, 0], ffn_w2[i, 0]), ln_g[i, 0], ln_b[i, 0])
        mix = _token_mixer(x, w_in[i], cmp_pos[i], cmp_w1[i], cmp_w2[i], ret_gn_g[i], ret_gn_b[i], w_out[i])
        x = _layer_norm(ALPHA * x + mix, ln_g[i, 1], ln_b[i, 1])
        x = _layer_norm(ALPHA * x + 0.5 * _swiglu(x, ffn_w13[i, 1], ffn_w2[i, 1]), ln_g[i, 2], ln_b[i, 2])
        e = (p[i] @ w_ple[i]) * jax.nn.sigmoid(x @ w_ple_gate[i])
        x = _layer_norm(ALPHA * x + e, ln_g[i, 3], ln_b[i, 3])
    return x
```

```python
import os
from contextlib import ExitStack
import numpy as np
import concourse.bass as bass
import concourse.mybir as mybir
from concourse.bass_utils import run_bass_kernel_spmd

F32 = mybir.dt.float32
BF16 = mybir.dt.bfloat16
AF = mybir.ActivationFunctionType
ALU = mybir.AluOpType
AX = mybir.AxisListType

D = 1024
DFF = 2816
NJ = 22
ALPHA = 2.0 ** 0.25
LN_EPS = 1e-5
EPS_LN = LN_EPS / (ALPHA * ALPHA)
NEG = -30000.0


class Inst:
    __slots__ = ("eng", "fn", "deps", "needed", "is_dma", "sem", "val", "idx")

    def __init__(self, eng, fn, is_dma=False):
        self.eng = eng
        self.fn = fn
        self.deps = set()
        self.needed = False
        self.is_dma = is_dma
        self.sem = None
        self.val = 0
        self.idx = 0


class Prog:
    ENGS = ("pe", "act", "dve", "pool", "sp")

    def __init__(self, nc, dma_ring=12, same_engine_sync=True):
        self.nc = nc
        self.insts = []
        self.res = {}
        self.dma_ring = dma_ring
        self.same_engine_sync = same_engine_sync

    def _track(self, inst, reads, writes):
        res = self.res
        ps_reads = [k for k in reads if isinstance(k, tuple) and k[0] == "ps"]
        if ps_reads:
            reads = [k for k in reads if k not in ps_reads]
            writes = list(writes) + [k for k in ps_reads if k not in writes]
        for k in reads:
            r = res.get(k)
            if r is None:
                r = res[k] = [None, []]
            if r[0] is not None:
                inst.deps.add(r[0])
            r[1].append(inst)
        for k in writes:
            r = res.get(k)
            if r is None:
                r = res[k] = [None, []]
            if r[0] is not None:
                inst.deps.add(r[0])
            for q in r[1]:
                if q is not inst:
                    inst.deps.add(q)
            r[0] = inst
            r[1] = []
        inst.deps.discard(inst)
        inst.idx = len(self.insts)
        self.insts.append(inst)
        return inst

    def op(self, eng, fn, reads=(), writes=()):
        return self._track(Inst(eng, fn), reads, writes)

    def dma(self, fn, reads=(), writes=(), queue="sp"):
        return self._track(Inst(queue, fn, is_dma=True), reads, writes)

    def emit(self, sems):
        nc = self.nc
        ring_cnt = {}
        ring_last = {}
        for inst in self.insts:
            if inst.is_dma:
                q = inst.eng
                n = ring_cnt.get(q, 0)
                ring_cnt[q] = n + 1
                slot = n % self.dma_ring
                inst.sem = sems["ring_%s_%d" % (q, slot)]
                inst.val = 16 * (n // self.dma_ring + 1)
                prev = ring_last.get((q, slot))
                if prev is not None:
                    inst.deps.add(prev)
                ring_last[(q, slot)] = inst
        for inst in self.insts:
            for d in inst.deps:
                if d.is_dma:
                    d.needed = True
                elif d.eng == inst.eng and (d.eng == "pe" or not self.same_engine_sync):
                    pass
                else:
                    d.needed = True
        cnt = {e: 0 for e in self.ENGS}
        for inst in self.insts:
            if inst.is_dma:
                inst.needed = True
                continue
            if inst.needed:
                cnt[inst.eng] += 1
                inst.sem = sems[inst.eng]
                inst.val = cnt[inst.eng]
        lists = {e: [] for e in self.ENGS}
        known = {e: {} for e in self.ENGS}
        for inst in self.insts:
            waits = {}
            kn = known[inst.eng]
            for d in inst.deps:
                if not d.is_dma and d.eng == inst.eng and (d.eng == "pe" or not self.same_engine_sync):
                    continue
                key = id(d.sem)
                if kn.get(key, 0) >= d.val:
                    continue
                if key not in waits or waits[key][1] < d.val:
                    waits[key] = (d.sem, d.val)
            for key, (s, v) in waits.items():
                kn[key] = v
            lists[inst.eng].append((list(waits.values()), inst))
        self.stats = dict(n_inst=len(self.insts), per_eng={e: len(lists[e]) for e in self.ENGS})

        def run(e, items):
            for waits, inst in items:
                for (s, v) in waits:
                    e.wait_ge(s, v)
                if inst.fn is None:
                    continue
                r = inst.fn(e)
                if inst.needed:
                    r.then_inc(inst.sem, 16 if inst.is_dma else 1)

        with nc.Block() as block:
            @block.tensor
            def _(e):
                run(e, lists["pe"])

            @block.scalar
            def _(e):
                run(e, lists["act"])

            @block.vector
            def _(e):
                run(e, lists["dve"])

            @block.gpsimd
            def _(e):
                run(e, lists["pool"])

            @block.sync
            def _(e):
                run(e, lists["sp"])


def _gammas():
    return 1.0 - np.exp2(-5.0 - np.arange(4, dtype=np.float64))


def make_consts(T, par):
    c = {}
    c["identf"] = np.eye(128, dtype=np.float32)
    c["onesf"] = np.full((128, 128), 1.0 / D, np.float32)
    c["i4"] = np.tile(np.eye(128, dtype=np.float32), (1, 4))
    ov = np.zeros((128, 4, 129), np.float32)
    for kt in range(4):
        for p in range(128):
            cc = kt * 128 + p
            for s in range(128):
                lo = max(16 * cc, 64 * s)
                hi = min(16 * cc + 32, 64 * s + 64)
                if hi > lo:
                    ov[p, kt, s] = (hi - lo) / 32.0
            ov[p, kt, 128] = 1.0
    c["ovm"] = ov.reshape(128, 4 * 129)
    r = np.arange(128)
    x = np.arange(384)
    hw = np.where(16 * (x[None, :] - 8 * par - 120) + 31 <= r[:, None], 0.0, NEG)
    c["hc"] = hw.astype(np.float32)
    hp = np.zeros((128, 128), np.float32)
    if par == 0:
        hp[:15, 127] = NEG
    c["hprev"] = hp
    y = np.arange(384)
    curq = (r >= 64).astype(np.int64)
    rel = y[None, :] - 2 * par - 128
    g = np.zeros((128, 384), np.float32)
    g[rel > curq[:, None]] = -1e9
    g[rel == curq[:, None]] = 1e9
    g[rel == curq[:, None] - 1] = 2e9
    c["gsel"] = g
    caus = np.where(r[None, :] <= r[:, None], 0.0, NEG).astype(np.float32)
    allm = np.full((128, 128), NEG, np.float32)
    zero = np.zeros((128, 128), np.float32)
    c["cmT"] = np.concatenate([caus, allm] if par == 0 else [zero, caus], axis=1)
    upper = np.where(r[None, :] > r[:, None], 0.0, NEG).astype(np.float32)
    if par == 0:
        wm = [upper, zero, caus, allm]
    else:
        wm = [allm, upper, zero, caus]
    c["wmT"] = np.concatenate(wm, axis=1)
    gam = _gammas()
    i = np.arange(128, dtype=np.float64)
    tri = (r[:, None] <= r[None, :]).astype(np.float32)
    c["tri4"] = np.tile(tri, (1, 4))
    facq = np.stack([gam[h] ** (i + 1.0) for h in range(4)], 0)
    fack = np.stack([128.0 ** -0.5 * gam[h] ** (-(i + 1.0)) for h in range(4)], 0)
    c["facq"] = np.tile(facq.reshape(1, 512), (128, 1)).astype(np.float32)
    c["fack"] = np.tile(fack.reshape(1, 512), (128, 1)).astype(np.float32)
    fkv = np.stack([128.0 ** -0.5 * gam[h] ** (127.0 - i) for h in range(4)], 1)
    c["fkv"] = fkv.astype(np.float32)
    pos = np.arange(T, dtype=np.float32)
    freqs = (10000.0 ** (-np.arange(0, 128, 2, dtype=np.float32) / 128.0)).astype(np.float32)
    ang = pos[:, None] * freqs[None, :]
    cs, sn = np.cos(ang).astype(np.float32), np.sin(ang).astype(np.float32)
    rot = np.concatenate([cs, cs, -sn, sn], axis=1).reshape(T // 128, 128, 256)
    c["rotk"] = np.ascontiguousarray(rot)
    c["rotq"] = np.ascontiguousarray(rot[par::2])
    sel = np.zeros((128, 2), np.float32)
    sel[:, par] = 1.0
    c["selc"] = sel
    return c


def layout_weights(inp):
    w = {}
    w13 = inp["ffn_w13"][0]
    w2 = inp["ffn_w2"][0]
    a = w13[:, :, :DFF].reshape(2, 8, 128, NJ, 128)
    u = w13[:, :, DFF:].reshape(2, 8, 128, NJ, 128)
    au = np.stack([a, u], axis=4)
    w["w13r"] = np.ascontiguousarray(au.transpose(0, 3, 2, 1, 4, 5)).reshape(2, NJ, 128, 8 * 256)
    w["w2r"] = np.ascontiguousarray(w2.reshape(2, NJ, 128, 8, 128).transpose(0, 3, 2, 1, 4)).reshape(2, 8, 128, NJ * 128)
    win = inp["w_in"][0]
    qcols = [np.concatenate([np.arange((0 * 4 + r) * 64, (0 * 4 + r) * 64 + 64),
                             np.arange((4 + r) * 64, (4 + r) * 64 + 64)]) for r in range(4)]
    fcols = qcols + [np.arange(768, 896), np.arange(1024, 1152), np.arange(512, 640), np.arange(640, 768)]
    winF = np.stack([win[:, cc] for cc in fcols], 0)
    w["winF"] = np.ascontiguousarray(winF.reshape(8, 8, 128, 128).transpose(0, 2, 1, 3)).reshape(8, 128, 1024)
    pad = lambda cols: np.concatenate([win[:, cols], np.zeros((D, 512 - len(cols)), np.float32)], axis=1)
    tcols = [np.concatenate([np.arange(896, 1024), np.arange(1152, 1280)]),
             np.arange(1816, 2328), np.arange(2328, 2840),
             np.arange(1280, 1304), np.arange(1304, 1816), np.arange(2840, 3352)]
    winT = np.stack([pad(cc) for cc in tcols], 0)
    w["winT"] = np.ascontiguousarray(winT.reshape(6, 8, 128, 512).transpose(0, 2, 1, 3)).reshape(6, 128, 4096)
    rl = lambda m, kcn: np.ascontiguousarray(m.reshape(kcn, 128, 8, 128).transpose(2, 1, 0, 3)).reshape(8, 128, kcn * 128)
    w["woutr"] = rl(inp["w_out"][0], 8)
    w["wgater"] = rl(inp["w_ple_gate"][0], 8)
    w["wpler"] = rl(inp["w_ple"][0], 2)
    cw1 = inp["cmp_w1"][0].reshape(2, 32, 64, 2, 128)
    zz = np.zeros_like(cw1)
    cw1 = np.stack([np.concatenate([cw1, zz], axis=2), np.concatenate([zz, cw1], axis=2)], axis=1)
    w["cw1r"] = np.ascontiguousarray(cw1.transpose(0, 1, 4, 3, 2, 5)).reshape(2, 2, 2, 128, 32 * 128)
    cw2 = inp["cmp_w2"][0].reshape(2, 2, 128, 64)
    cw2 = np.concatenate([cw2, cw2], axis=3)
    w["cw2d"] = np.ascontiguousarray(cw2.transpose(2, 0, 1, 3)).reshape(128, 512)
    pos = inp["cmp_pos"][0]
    posT = np.concatenate([pos, pos], axis=2).transpose(2, 0, 1)
    w["posT"] = np.ascontiguousarray(posT).reshape(128, 64)
    lng = inp["ln_g"][0].reshape(4, 8, 128).transpose(2, 0, 1)
    lnb = inp["ln_b"][0].reshape(4, 8, 128).transpose(2, 0, 1)
    w["lnp"] = np.ascontiguousarray(np.stack([lng, lnb], 1)).reshape(128, 64)
    gn = np.stack([inp["ret_gn_g"][0], inp["ret_gn_b"][0]], 0).reshape(1, 1024)
    w["gnp"] = np.ascontiguousarray(np.tile(gn, (128, 1)))
    return w


class _Stop(Exception):
    pass


def build_nc(T, dbg=False, stop=0):
    NB = T // 512
    NT = T // 128
    TO = T // 2
    gam = _gammas()
    nc = bass.Bass("TRN2", target_bir_lowering=False)
    P = Prog(nc)
    din = {}

    def dram_in(name, shape):
        din[name] = nc.dram_tensor(name, list(shape), F32, kind="ExternalInput").ap()
        return din[name]

    xT_d = dram_in("xT", (8, 128, T))
    pT_d = dram_in("pT", (2, 128, TO))
    wshapes = dict(w13r=(2, NJ, 128, 2048), w2r=(2, 8, 128, NJ * 128), winF=(8, 128, 1024), winT=(6, 128, 4096),
                   woutr=(8, 128, 1024), wgater=(8, 128, 1024), wpler=(8, 128, 256), cw1r=(2, 2, 2, 128, 4096))
    wsrc = {k: dram_in(k, s) for k, s in wshapes.items()}
    wscr = {k: nc.dram_tensor(k + "_bf", list(s), BF16, kind="Internal").ap() for k, s in wshapes.items()}
    small_f32 = dict(lnp=64, gnp=1024, fack=512, facq=512, fkv=4, selc=2, gsel=384, identf=128, onesf=128)
    small_bf = dict(cw2d=512, posT=64, i4=512, ovm=516, hc=384, hprev=128, cmT=256, wmT=512, tri4=512)
    for k, n in list(small_f32.items()) + list(small_bf.items()):
        dram_in(k, (128, n))
    rotk_d = dram_in("rotk", (NT, 128, 256))
    rotq_d = dram_in("rotq", (NT // 2, 128, 256))
    out_d = nc.dram_tensor("outT", [8, 128, TO], F32, kind="ExternalOutput").ap()
    if dbg:
        dbg_d = nc.dram_tensor("dbg", [16, 128, 4096], F32, kind="ExternalOutput").ap()

    with ExitStack() as es:
        def sb(name, cols, dt=F32):
            return es.enter_context(nc.sbuf_tensor("sb_" + name, [128, cols], dt))

        sems = {}
        for e in Prog.ENGS:
            sems[e] = es.enter_context(nc.semaphore("s_" + e))
        for q in ("sp", "pool"):
            for i in range(P.dma_ring):
                sems["ring_%s_%d" % (q, i)] = es.enter_context(nc.semaphore("r_%s_%d" % (q, i)))
        ps = [es.enter_context(nc.psum_tensor("ps%d" % i, [128, 512], F32)) for i in range(8)]
        rr = [0]

        def nxt():
            rr[0] = (rr[0] + 1) % 4
            return rr[0]

        def pk(b):
            return ("ps", b)

        def MM(out, lhsT, rhs, start, stop, r, w):
            P.op("pe", lambda e: e.matmul(out, lhsT=lhsT, rhs=rhs, start=start, stop=stop, skip_group_check=True),
                 reads=r, writes=w)

        def TR(out, in_, ident, r, w):
            P.op("pe", lambda e: e.transpose(out=out, in_=in_, identity=ident), reads=r, writes=w)

        def ACT(out, in_, func, r, w, scale=1.0):
            P.op("act", lambda e: e.activation(out=out, in_=in_, func=func, scale=scale), reads=r, writes=w)

        def CP(eng, out, in_, r, w):
            if eng == "act":
                P.op("act", lambda e: e.copy(out=out, in_=in_), reads=r, writes=w)
            else:
                P.op(eng, lambda e: e.tensor_copy(out=out, in_=in_), reads=r, writes=w)

        def TT(eng, out, in0, in1, op, r, w):
            P.op(eng, lambda e: e.tensor_tensor(out=out, in0=in0, in1=in1, op=op), reads=r, writes=w)

        def TS(eng, out, in0, s1, s2, op0, op1, r, w):
            if op1 is None:
                P.op(eng, lambda e: e.tensor_scalar(out=out, in0=in0, scalar1=s1, scalar2=None, op0=op0), reads=r, writes=w)
            else:
                P.op(eng, lambda e: e.tensor_scalar(out=out, in0=in0, scalar1=s1, scalar2=s2, op0=op0, op1=op1), reads=r, writes=w)

        def STT(eng, out, in0, scalar, in1, op0, op1, r, w):
            P.op(eng, lambda e: e.scalar_tensor_tensor(out=out, in0=in0, scalar=scalar, in1=in1, op0=op0, op1=op1),
                 reads=r, writes=w)

        def MEMSET(eng, ap, val, w):
            P.op(eng, lambda e: e.memset(ap, val), writes=w)

        def DMA(out, in_, r, w, queue="sp"):
            P.dma(lambda e: e.dma_start(out=out, in_=in_), reads=r, writes=w, queue=queue)

        dbg_n = [0]

        def DBG(ap, r, cols):
            if not dbg:
                return
            s = dbg_n[0]
            dbg_n[0] += 1
            DMA(dbg_d[s, 0:ap.shape[0], 0:cols], ap, r, [("dbg", s)])
            return s

        def v3(ap, a):
            return ap.rearrange("p (a b) -> p a b", a=a)

        def bc_mid(ap, n):
            return ap.unsqueeze(1).to_broadcast([ap.shape[0], n, ap.shape[1]])

        def bc_last(ap, n):
            return ap.unsqueeze(2).to_broadcast([ap.shape[0], ap.shape[1], n])

        xT = sb("xT", 4096)
        xb = sb("xb", 4096, BF16)
        gT = sb("gT", NJ * 512, BF16)
        selx = gT
        scr = [sb("scr%d" % i, 512) for i in range(5)]
        sc = [0]

        def nscr():
            sc[0] = (sc[0] + 1) % 5
            return sc[0]

        NW = 4
        wbuf = [sb("wbuf%d" % i, 2048, BF16) for i in range(NW)]
        wc = [0]

        def wslot():
            wc[0] = (wc[0] + 1) % NW
            return wc[0]

        Ksel = sb("Ksel", T, BF16)
        Vsel = sb("Vsel", NT * 132, BF16)
        Kwin = sb("Kwin", 1024, BF16)
        Vwin = sb("Vwin", 8 * 132, BF16)
        KcT = sb("KcT", 512, BF16)
        VcT = sb("VcT", 512, BF16)
        Vc = sb("Vc", 2 * 4 * 66, BF16)
        KR = sb("KR", 528, BF16)
        VR = sb("VR", 528, BF16)
        xo = sb("xo", 4096)
        xob = sb("xob", 4096, BF16)
        mixTp = sb("mixTp", 1024, BF16)
        pTb = sb("pTb", 1024, BF16)
        krot = sb("krot", 2048, BF16)
        vrb = sb("vrb", 2048, BF16)
        vtil = sb("vtil", 2048, BF16)
        Sst = [sb("Sst%d" % i, 512) for i in range(3)]
        rk = [sb("rk%d" % i, 256) for i in range(2)]
        rq = sb("rq", 256)
        QA = [sb("QA%d" % i, 512, BF16) for i in range(2)]
        gates = sb("gates", 32)
        qrot = sb("qrot", 512, BF16)
        sgr = sb("sgr", 512)
        krown = sb("krown", 512, BF16)
        vrown = sb("vrown", 512, BF16)
        Sown = [sb("Sown%d" % i, 512, BF16) for i in range(2)]
        kTt = sb("kTt", 512, BF16)
        qTt = sb("qTt", 512, BF16)
        ATt = sb("ATt", 512, BF16)
        mixr = sb("mixr", 512, BF16)
        PT = [sb("PT%d" % i, 512, BF16) for i in range(3)]
        ptc = [0]
        obr = sb("obr", 512)
        us = sb("us", 516)
        acc = sb("acc", 128)
        accw = sb("accw", 128)
        m8 = sb("m8", 16)
        selb = sb("selb", 128, BF16)
        sm = sb("sm", 64)
        onsa = sb("onsa", 512)
        onsab = sb("onsab", 512, BF16)
        hx = sb("hx", 256)
        hgl = sb("hgl", 256, BF16)
        cb = sb("cb", 4)
        cf = {k: sb("c_" + k, n) for k, n in small_f32.items()}
        cbf = {k: sb("c_" + k, n, BF16) for k, n in small_bf.items()}
        identb = sb("identb", 128, BF16)
        onesb = sb("onesb", 128, BF16)
        lnbf = [PT[0], PT[1], PT[2], ATt]
        lnk = [("PT", 0), ("PT", 1), ("PT", 2), "ATt"]

        for k in small_f32:
            DMA(cf[k][:], din[k], [], ["c_" + k])
        for k in small_bf:
            DMA(cbf[k][:], din[k], [], ["c_" + k], queue="pool")
        DMA(identb[:], din["identf"], [], ["identb"], queue="pool")
        DMA(onesb[:], din["onesf"], [], ["onesb"], queue="pool")

        def cast_w(name, idx):
            src = wsrc[name]
            dst = wscr[name]
            for i in idx:
                src, dst = src[i], dst[i]
            DMA(dst, src, [], [(name,) + tuple(idx)], queue="pool")

        for j in range(NJ):
            cast_w("w13r", (0, j))
        for oc in range(8):
            cast_w("w2r", (0, oc))
        for fc in range(8):
            cast_w("winF", (fc,))
        for kv in range(2):
            for g in range(2):
                for hcn in range(2):
                    cast_w("cw1r", (kv, g, hcn))
        for s in range(6):
            cast_w("winT", (s,))
        for oc in range(8):
            cast_w("woutr", (oc,))
        for j in range(NJ):
            cast_w("w13r", (1, j))
        for oc in range(8):
            cast_w("w2r", (1, oc))
        for oc in range(8):
            cast_w("wgater", (oc,))
        for oc in range(8):
            cast_w("wpler", (oc,))

        def load_w(name, idx, cols, part=0, nparts=1, slot=None):
            s = wslot() if slot is None else slot
            src = wscr[name]
            for i in idx:
                src = src[i]
            pc = cols // nparts
            DMA(wbuf[s][:, 0:pc], src[:, part * pc:(part + 1) * pc], [(name,) + tuple(idx)], [("wbuf", s)])
            return s

        def load_parts(name, idx, cols, nparts):
            return [load_w(name, idx, cols, p, nparts) for p in range(nparts)]

        MEMSET("pool", KcT[:], 0.0, ["KcT"])
        MEMSET("pool", VcT[:], 0.0, ["VcT"])
        MEMSET("pool", Vc[:], 1.0, ["Vc"])
        MEMSET("pool", Vsel[:], 1.0, ["Vsel"])
        MEMSET("pool", Vwin[:], 1.0, ["Vwin"])
        MEMSET("pool", KR[:], 0.0, ["KR"])
        MEMSET("pool", VR[:], 0.0, ["VR"])
        MEMSET("pool", Sst[0][:], 0.0, [("Sst", 0)])
        MEMSET("pool", Kwin[:], 0.0, ["Kwin"])
        MEMSET("pool", QA[0][:], 0.0, [("QA", 0)])
        MEMSET("pool", QA[1][:], 0.0, [("QA", 1)])

        cbB = nxt()
        for kv in range(2):
            for hcn in range(2):
                pts = load_parts("cw1r", (kv, 0, hcn), 4096, 2)
                for l in range(32):
                    s = pts[l // 16]
                    MM(ps[cbB][:, kv * 2 + hcn:kv * 2 + hcn + 1], v3(wbuf[s][:, :], 16)[:, l % 16, :], cbf["posT"][:, kv * 32 + l:kv * 32 + l + 1],
                       l == 0, l == 31, [("wbuf", s), "c_posT"], [pk(cbB)])
        CP("dve", cb[:, 0:4], ps[cbB][:, 0:4], [pk(cbB)], ["cb"])

        lnp = cf["lnp"]

        def LN(z, zb, n, zk, zbk):
            MB, QB = nxt(), nxt()
            for c in range(8):
                s = (2 * c) % 4
                ACT(lnbf[s][:], z[:, c * 512:(c + 1) * 512], AF.Square, [zk], [lnk[s]])
                CP("act", lnbf[s + 1][:], z[:, c * 512:(c + 1) * 512], [zk], [lnk[s + 1]])
                MM(ps[MB][:], onesb[:], lnbf[s + 1][:], c == 0, c == 7, ["onesb", lnk[s + 1]], [pk(MB)])
                MM(ps[QB][:], onesb[:], lnbf[s][:], c == 0, c == 7, ["onesb", lnk[s]], [pk(QB)])
            sm_, s2_, sr_ = nscr(), nscr(), nscr()
            CP("act", scr[sm_][:], ps[MB][:], [pk(MB)], [("scr", sm_)])
            TT("pool", scr[s2_][:], scr[sm_][:], scr[sm_][:], ALU.mult, [("scr", sm_)], [("scr", s2_)])
            TT("dve", scr[sr_][:], ps[QB][:], scr[s2_][:], ALU.subtract, [pk(QB), ("scr", s2_)], [("scr", sr_)])
            TS("dve", scr[sr_][:], scr[sr_][:], EPS_LN, None, ALU.add, None, [("scr", sr_)], [("scr", sr_)])
            ACT(scr[sr_][:], scr[sr_][:], AF.Sqrt, [("scr", sr_)], [("scr", sr_)])
            P.op("dve", lambda e, sr_=sr_: e.reciprocal(out=scr[sr_][:], in_=scr[sr_][:]), reads=[("scr", sr_)], writes=[("scr", sr_)])
            for c in range(8):
                zc = z[:, c * 512:(c + 1) * 512]
                a = nscr()
                while a in (sm_, sr_):
                    a = nscr()
                TT("pool", scr[a][:], zc, scr[sm_][:], ALU.subtract, [zk, ("scr", sm_)], [("scr", a)])
                TT("dve", scr[a][:], scr[a][:], scr[sr_][:], ALU.mult, [("scr", a), ("scr", sr_)], [("scr", a)])
                TS("dve", zc, scr[a][:], lnp[:, n * 8 + c:n * 8 + c + 1], lnp[:, 32 + n * 8 + c:32 + n * 8 + c + 1],
                   ALU.mult, ALU.add, [("scr", a), "c_lnp"], [zk])
                CP("act", zb[:, c * 512:(c + 1) * 512], zc, [zk], [zbk])

        def FFN(f, z, zb, zk, zbk):
            for j in range(NJ):
                s = load_w("w13r", (f, j), 2048)
                slab = v3(wbuf[s][:, 0:2048], 8)
                A, U = nxt(), nxt()
                for kc in range(8):
                    MM(ps[A][:], slab[:, kc, 0:128], zb[:, kc * 512:(kc + 1) * 512], kc == 0, kc == 7, [("wbuf", s), zbk], [pk(A)])
                for kc in range(8):
                    MM(ps[U][:], slab[:, kc, 128:256], zb[:, kc * 512:(kc + 1) * 512], kc == 0, kc == 7, [("wbuf", s), zbk], [pk(U)])
                t = nscr()
                ACT(scr[t][:], ps[A][:], AF.Silu, [pk(A)], [("scr", t)])
                TT("dve", gT[:, j * 512:(j + 1) * 512], scr[t][:], ps[U][:], ALU.mult, [("scr", t), pk(U)], [("gT", j)])
            for oc in range(8):
                pts = load_parts("w2r", (f, oc), NJ * 128, 2)
                Y = nxt()
                for kc in range(NJ):
                    s = pts[kc // 11]
                    MM(ps[Y][:], v3(wbuf[s][:, 0:1408], 11)[:, kc % 11, :], gT[:, kc * 512:(kc + 1) * 512], kc == 0, kc == NJ - 1,
                       [("wbuf", s), ("gT", kc)], [pk(Y)])
                zc = z[:, oc * 512:(oc + 1) * 512]
                STT("dve", zc, ps[Y][:], 0.5 / ALPHA, zc, ALU.mult, ALU.add, [pk(Y), zk], [zk])

        def ROT(pb, tab, tabk, dst, dstk):
            a, b = nscr(), nscr()
            pv = v3(ps[pb][:], 4)
            TT("dve", v3(scr[a][:], 4), pv, bc_mid(tab[:, 0:128], 4), ALU.mult, [pk(pb), tabk], [("scr", a)])
            TT("dve", v3(scr[b][:], 4)[:, :, 0:64], pv[:, :, 64:128], bc_mid(tab[:, 128:192], 4), ALU.mult, [pk(pb), tabk], [("scr", b)])
            TT("dve", v3(scr[b][:], 4)[:, :, 64:128], pv[:, :, 0:64], bc_mid(tab[:, 192:256], 4), ALU.mult, [pk(pb), tabk], [("scr", b)])
            TT("pool", dst, scr[a][:], scr[b][:], ALU.add, [("scr", a), ("scr", b)], [dstk])

        def TR4(src, srck, bank):
            for c in range(4):
                MM(ps[bank][:, c * 128:(c + 1) * 128], src[:, c * 128:(c + 1) * 128], identb[:], True, True, [srck, "identb"], [pk(bank)])

        BK_O, BK_U0, BK_U1, BK_T = 7, 5, 6, 4

        def STOP(k):
            if stop == k:
                raise _Stop()

        def main_loop():
          for m in range(NB):
              if m == 0:
                  DMA(v3(xT[:], 8), xT_d[:, :, m * 512:(m + 1) * 512].rearrange("c p t -> p c t"), [], ["xT"])
              CP("act", xb[:, 0:2048], xT[:, 0:2048], ["xT"], ["xb"])
              CP("pool", xb[:, 2048:4096], xT[:, 2048:4096], ["xT"], ["xb"])
              FFN(0, xT, xb, "xT", "xb")
              LN(xT, xb, 0, "xT", "xb")
              if dbg and m == 0:
                  DBG(xT[:], ["xT"], 4096)
              for jj in range(2):
                  j = 2 * m + jj
                  oc0 = (j % 4) * 128
                  cA, cB = (2 * jj) * 128, (2 * jj + 1) * 128
                  xo3, xT3, xob3 = v3(xo[:], 8), v3(xT[:], 8), v3(xob[:], 8)
                  a = nscr()
                  TS("pool", v3(scr[a][:], 8)[:, :, 0:64], xT3[:, :, cA:cA + 64], cf["selc"][:, 0:1], None, ALU.mult, None, ["xT", "c_selc"], [("scr", a)])
                  b = nscr()
                  TS("pool", v3(scr[b][:], 8)[:, :, 0:64], xT3[:, :, cA + 64:cA + 128], cf["selc"][:, 0:1], None, ALU.mult, None, ["xT", "c_selc"], [("scr", b)])
                  STT("dve", xo3[:, :, oc0:oc0 + 64], xT3[:, :, cB:cB + 64], cf["selc"][:, 1:2], v3(scr[a][:], 8)[:, :, 0:64], ALU.mult, ALU.add,
                      ["xT", "c_selc", ("scr", a)], ["xo"])
                  STT("dve", xo3[:, :, oc0 + 64:oc0 + 128], xT3[:, :, cB + 64:cB + 128], cf["selc"][:, 1:2], v3(scr[b][:], 8)[:, :, 0:64], ALU.mult, ALU.add,
                      ["xT", "c_selc", ("scr", b)], ["xo"])
                  CP("act", xob3[:, :, oc0:oc0 + 128], xo3[:, :, oc0:oc0 + 128], ["xo"], ["xob"])
              if m + 1 < NB:
                  DMA(v3(xT[:], 8), xT_d[:, :, (m + 1) * 512:(m + 2) * 512].rearrange("c p t -> p c t"), [], ["xT"])
              STOP(1)
              for fc in (4, 5, 6, 7):
                  s = load_w("winF", (fc,), 1024)
                  slab = v3(wbuf[s][:, 0:1024], 8)
                  B = nxt()
                  for kc in range(8):
                      MM(ps[B][:], slab[:, kc, :], xb[:, kc * 512:(kc + 1) * 512], kc == 0, kc == 7, [("wbuf", s), "xb"], [pk(B)])
                  if fc == 4:
                      CP("act", Ksel[:, m * 512:(m + 1) * 512], ps[B][:], [pk(B)], ["Ksel"])
                  elif fc == 5:
                      CP("dve", Kwin[:, (m % 2) * 512:(m % 2) * 512 + 512], ps[B][:], [pk(B)], ["Kwin"])
                  else:
                      R_, rkey = (KR, "KR") if fc == 6 else (VR, "VR")
                      CP("pool", R_[:, 0:16], R_[:, 512:528], [rkey], [rkey])
                      CP("act" if fc == 6 else "dve", R_[:, 16:528], ps[B][:], [pk(B)], [rkey])
              STOP(2)
              H = BK_T
              cparts = []
              for kv in range(2):
                  for hcn in range(2):
                      for g in range(2):
                          for p_ in range(2):
                              def cpart(kv=kv, hcn=hcn, g=g, p_=p_, ci=len(cparts)):
                                  R_, rkey = (KR, "KR") if kv == 0 else (VR, "VR")
                                  s = load_w("cw1r", (kv, g, hcn), 4096, p_, 2, slot=2 + ci % 2)
                                  c0 = kv * 128 + g * 64 + hcn * 32
                                  for l in range(16 * p_, 16 * p_ + 16):
                                      MM(ps[H][:, c0:c0 + 32], v3(wbuf[s][:, :], 16)[:, l % 16, :], R_[:, l:l + 497:16],
                                         l == 0, l == 31, [("wbuf", s), rkey], [pk(H)])
                              cparts.append(cpart)
              STOP(3)
              for slab_i in range(3):
                  STOP(31 + slab_i)
                  pts = [load_w("winT", (slab_i,), 4096, 0, 2, slot=0), load_w("winT", (slab_i,), 4096, 1, 2, slot=1)]
                  for tt in range(4):
                      if cparts:
                          cparts.pop(0)()
                      n = 4 * m + tt
                      B = nxt()
                      ncol = 256 if slab_i == 0 else 512
                      for kc in range(8):
                          s = pts[kc // 4]
                          MM(ps[B][:, 0:ncol], xb[:, kc * 512 + tt * 128:kc * 512 + tt * 128 + 128], v3(wbuf[s][:, :], 4)[:, kc % 4, 0:ncol],
                             kc == 0, kc == 7, ["xb", ("wbuf", s)], [pk(B)])
                      if slab_i == 0 and os.environ.get("KSKIP") == "1":
                          pass
                      elif slab_i == 0:
                          for g in range(2):
                              if os.environ.get("KSKIP") != "2":
                                  CP("act", Vsel[:, n * 132 + g * 66:n * 132 + g * 66 + 64], ps[B][:, g * 64:(g + 1) * 64], [pk(B)], ["Vsel"])
                              if os.environ.get("KSKIP") == "3":
                                  continue
                              CP("dve", Vwin[:, (n % 8) * 132 + g * 66:(n % 8) * 132 + g * 66 + 64], ps[B][:, 128 + g * 64:128 + (g + 1) * 64],
                                 [pk(B)], ["Vwin"])
                      elif slab_i == 1:
                          DMA(rk[tt % 2][:], rotk_d[n], [], [("rk", tt % 2)])
                          ROT(B, rk[tt % 2], ("rk", tt % 2), krot[:, tt * 512:(tt + 1) * 512], ("krot", tt))
                      else:
                          CP("act", vrb[:, tt * 512:(tt + 1) * 512], ps[B][:], [pk(B)], [("vrb", tt)])
                          TT("dve", v3(vtil[:, tt * 512:(tt + 1) * 512], 4), v3(ps[B][:], 4), bc_last(cf["fkv"][:, 0:4], 128), ALU.mult,
                             [pk(B), "c_fkv"], [("vtil", tt)])
              STOP(34)
              for tt in range(4):
                  if cparts:
                      cparts.pop(0)()
                  n = 4 * m + tt
                  B = nxt()
                  for h in range(4):
                      MM(ps[B][:, h * 128:(h + 1) * 128], krot[:, tt * 512 + h * 128:tt * 512 + h * 128 + 128],
                         vtil[:, tt * 512 + h * 128:tt * 512 + h * 128 + 128], True, True, [("krot", tt), ("vtil", tt)], [pk(B)])
                  if tt % 2 == 1:
                      a = nscr()
                      TS("pool", scr[a][:], Sst[(n - 1) % 3][:], cf["selc"][:, 0:1], None, ALU.mult, None, [("Sst", (n - 1) % 3), "c_selc"], [("scr", a)])
                      STT("dve", Sown[tt // 2][:], Sst[n % 3][:], cf["selc"][:, 1:2], scr[a][:], ALU.mult, ALU.add,
                          [("Sst", n % 3), "c_selc", ("scr", a)], [("Sown", tt // 2)])
                  for h in range(4):
                      hs = slice(h * 128, (h + 1) * 128)
                      STT("dve", Sst[(n + 1) % 3][:, hs], Sst[n % 3][:, hs], float(gam[h] ** 128.0), ps[B][:, hs], ALU.mult, ALU.add,
                          [("Sst", n % 3), pk(B)], [("Sst", (n + 1) % 3)])
              while cparts:
                  cparts.pop(0)()
              STOP(21)
              for kv in range(2):
                  for hcn in range(2):
                      pv = ps[H][:, kv * 128:(kv + 1) * 128].rearrange("p (g h c) -> p g h c", g=2, h=2)[:, :, hcn, :]
                      hv = hx[:, kv * 128:(kv + 1) * 128].rearrange("p (g h c) -> p g h c", g=2, h=2)[:, :, hcn, :]
                      TS("dve", hv, pv, cb[:, kv * 2 + hcn:kv * 2 + hcn + 1], None, ALU.add, None, [pk(H), "cb"], ["hx"])
              STOP(22)
              a, b = nscr(), nscr()
              ACT(scr[a][:, 0:256], hx[:], AF.Square, ["hx"], [("scr", a)])
              TS("dve", scr[a][:, 0:256], scr[a][:, 0:256], 0.044715, 1.0, ALU.mult, ALU.add, [("scr", a)], [("scr", a)])
              TT("pool", scr[a][:, 0:256], scr[a][:, 0:256], hx[:], ALU.mult, [("scr", a), "hx"], [("scr", a)])
              ACT(scr[b][:, 0:256], scr[a][:, 0:256], AF.Sigmoid, [("scr", a)], [("scr", b)], scale=1.5957691216057308)
              TT("dve", hgl[:], scr[b][:, 0:256], hx[:], ALU.mult, [("scr", b), "hx"], ["hgl"])
              STOP(23)
              OB2 = nxt()
              for kv in range(2):
                  for g in range(2):
                      for hcn in range(2):
                          c0 = kv * 128 + g * 64 + hcn * 32
                          MM(ps[OB2][:, (kv * 2 + g) * 32:(kv * 2 + g) * 32 + 32], cbf["cw2d"][:, (kv * 2 + hcn) * 128:(kv * 2 + hcn) * 128 + 128],
                             hgl[:, c0:c0 + 32], hcn == 0, hcn == 1, ["c_cw2d", "hgl"], [pk(OB2)])
              for kv in range(2):
                  dstT, dk = (KcT, "KcT") if kv == 0 else (VcT, "VcT")
                  for g in range(2):
                      sk = 1 if m == 0 else 0
                      c_lo = 32 * m - 1 + sk
                      CP("dve" if g == 0 else "act", dstT[g * 64:(g + 1) * 64, c_lo:32 * m + 31],
                         ps[OB2][g * 64:(g + 1) * 64, (kv * 2 + g) * 32 + sk:(kv * 2 + g) * 32 + 32], [pk(OB2)], [dk])
              STOP(24)
              kts = [m // 4] + ([m // 4 - 1] if (m % 4 == 0 and m > 0) else [])
              for kt in kts:
                  TBk = nxt()
                  MM(ps[TBk][:, 0:128], VcT[:, kt * 128:(kt + 1) * 128], identb[:], True, True, ["VcT", "identb"], [pk(TBk)])
                  for g in range(2):
                      CP("dve", Vc[:, (g * 4 + kt) * 66:(g * 4 + kt) * 66 + 64], ps[TBk][:, g * 64:(g + 1) * 64], [pk(TBk)], ["Vc"])
              STOP(4)
              for jj in range(2):
                  j = 2 * m + jj
                  oc0 = (j % 4) * 128
                  cA, cB = (2 * jj) * 128, (2 * jj + 1) * 128
                  xo3, xT3, xob3 = v3(xo[:], 8), v3(xT[:], 8), v3(xob[:], 8)
                  QB = nxt()
                  for r_ in range(4):
                      s = load_w("winF", (r_,), 1024)
                      slab = v3(wbuf[s][:, 0:1024], 8)
                      for kc in range(8):
                          MM(ps[QB][:, r_ * 128:(r_ + 1) * 128], slab[:, kc, :], xob[:, kc * 512 + oc0:kc * 512 + oc0 + 128],
                             kc == 0, kc == 7, [("wbuf", s), "xob"], [pk(QB)])
                  CP("act", QA[0][0:64, :], ps[QB][0:64, :], [pk(QB)], [("QA", 0)])
                  CP("dve", QA[1][64:128, :], ps[QB][64:128, :], [pk(QB)], [("QA", 1)])
                  DMA(rq[:], rotq_d[j], [], ["rq"])
                  for slab_i in (3, 4, 5):
                      pts = load_parts("winT", (slab_i,), 4096, 2)
                      B = nxt()
                      ncol = 32 if slab_i == 3 else 512
                      for kc in range(8):
                          s = pts[kc // 4]
                          MM(ps[B][:, 0:ncol], xob[:, kc * 512 + oc0:kc * 512 + oc0 + 128], v3(wbuf[s][:, :], 4)[:, kc % 4, 0:ncol], kc == 0, kc == 7,
                             ["xob", ("wbuf", s)], [pk(B)])
                      if slab_i == 3:
                          ACT(gates[:], ps[B][:, 0:32], AF.Sigmoid, [pk(B)], ["gates"])
                      elif slab_i == 4:
                          ROT(B, rq, "rq", qrot[:], "qrot")
                      else:
                          ACT(sgr[:], ps[B][:], AF.Silu, [pk(B)], ["sgr"])
                  STOP(5)
                  tA, tB = 2 * jj, 2 * jj + 1
                  for (src, skey, dst, dkey) in ((krot, "krot", krown, "krown"), (vrb, "vrb", vrown, "vrown")):
                      a = nscr()
                      TS("pool", scr[a][:], src[:, tA * 512:(tA + 1) * 512], cf["selc"][:, 0:1], None, ALU.mult, None, [(skey, tA), "c_selc"], [("scr", a)])
                      STT("dve", dst[:], src[:, tB * 512:(tB + 1) * 512], cf["selc"][:, 1:2], scr[a][:], ALU.mult, ALU.add,
                          [(skey, tB), "c_selc", ("scr", a)], [dkey])
                  B = nxt()
                  TR4(krown, "krown", B)
                  TT("dve", kTt[:], ps[B][:], cf["fack"][:], ALU.mult, [pk(B), "c_fack"], ["kTt"])
                  B = nxt()
                  TR4(qrot, "qrot", B)
                  TT("dve", qTt[:], ps[B][:], cf["facq"][:], ALU.mult, [pk(B), "c_facq"], ["qTt"])
                  B = nxt()
                  for h in range(4):
                      hs = slice(h * 128, (h + 1) * 128)
                      MM(ps[B][:, hs], kTt[:, hs], qTt[:, hs], True, True, ["kTt", "qTt"], [pk(B)])
                  TT("dve", ATt[:], ps[B][:], cbf["tri4"][:], ALU.mult, [pk(B), "c_tri4"], ["ATt"])
                  B = nxt()
                  for h in range(4):
                      hs = slice(h * 128, (h + 1) * 128)
                      MM(ps[B][:, hs], ATt[:, hs], vrown[:, hs], True, False, ["ATt", "vrown"], [pk(B)])
                      MM(ps[B][:, hs], qTt[:, hs], Sown[jj][:, hs], False, True, ["qTt", ("Sown", jj)], [pk(B)])
                  o_, q_ = nscr(), nscr()
                  CP("act", scr[o_][:], ps[B][:], [pk(B)], [("scr", o_)])
                  ACT(scr[q_][:], ps[B][:], AF.Square, [pk(B)], [("scr", q_)])
                  P.op("dve", lambda e, o_=o_: e.reduce_sum(out=sm[:, 0:4], in_=v3(scr[o_][:], 4), axis=AX.X), reads=[("scr", o_)], writes=["sm0"])
                  P.op("dve", lambda e, q_=q_: e.reduce_sum(out=sm[:, 4:8], in_=v3(scr[q_][:], 4), axis=AX.X), reads=[("scr", q_)], writes=["sm1"])
                  TS("dve", sm[:, 0:4], sm[:, 0:4], 1.0 / 128.0, None, ALU.mult, None, ["sm0"], ["sm0"])
                  TT("dve", sm[:, 8:12], sm[:, 0:4], sm[:, 0:4], ALU.mult, ["sm0"], ["sm2"])
                  STT("dve", sm[:, 4:8], sm[:, 4:8], 1.0 / 128.0, sm[:, 8:12], ALU.mult, ALU.subtract, ["sm1", "sm2"], ["sm1"])
                  TS("dve", sm[:, 4:8], sm[:, 4:8], LN_EPS, None, ALU.add, None, ["sm1"], ["sm1"])
                  ACT(sm[:, 4:8], sm[:, 4:8], AF.Sqrt, ["sm1"], ["sm1"])
                  P.op("dve", lambda e: e.reciprocal(out=sm[:, 4:8], in_=sm[:, 4:8]), reads=["sm1"], writes=["sm1"])
                  TT("dve", v3(scr[o_][:], 4), v3(scr[o_][:], 4), bc_last(sm[:, 0:4], 128), ALU.subtract, [("scr", o_), "sm0"], [("scr", o_)])
                  TT("dve", v3(scr[o_][:], 4), v3(scr[o_][:], 4), bc_last(sm[:, 4:8], 128), ALU.mult, [("scr", o_), "sm1"], [("scr", o_)])
                  TT("pool", scr[o_][:], scr[o_][:], cf["gnp"][:, 0:512], ALU.mult, [("scr", o_), "c_gnp"], [("scr", o_)])
                  TT("pool", scr[o_][:], scr[o_][:], cf["gnp"][:, 512:1024], ALU.add, [("scr", o_), "c_gnp"], [("scr", o_)])
                  TT("dve", mixr[:], scr[o_][:], sgr[:], ALU.mult, [("scr", o_), "sgr"], ["mixr"])
                  if dbg and j == 0:
                      DBG(scr[o_][:], [("scr", o_)], 512)
                  STOP(6)
                  for g in range(2):
                      rows = slice(g * 64, (g + 1) * 64)

                      def score(lhsT, lk, maskT, mk):
                          S = nxt()
                          MM(ps[S][:], lhsT, QA[g][:], True, maskT is None, [lk, ("QA", g)], [pk(S)])
                          if maskT is not None:
                              MM(ps[S][:], maskT, cbf["i4"][:], False, True, [mk, "c_i4"], [pk(S)])
                          ptc[0] = (ptc[0] + 1) % 3
                          pi = ptc[0]
                          ACT(PT[pi][:], ps[S][:], AF.Exp, [pk(S)], [("PT", pi)], scale=0.125)
                          return pi

                      og = v3(onsa[:, g * 256:(g + 1) * 256], 4)

                      def combine(br):
                          CP("act", obr[0:65, :], ps[BK_O][0:65, :], [pk(BK_O)], ["obr"])
                          for h in range(4):
                              TR(ps[BK_T][:, h * 65:(h + 1) * 65], obr[0:65, h * 128:(h + 1) * 128], cf["identf"][0:65, 0:65],
                                 ["obr", "c_identf"], [pk(BK_T)])
                          t3 = v3(ps[BK_T][:, 0:260], 4)
                          smv = sm[:, 20:24].unsqueeze(2)
                          TS("dve", smv, t3[:, :, 64:65], 1e-30, None, ALU.max, None, [pk(BK_T)], ["sm5"])
                          P.op("dve", lambda e: e.reciprocal(out=sm[:, 20:24], in_=sm[:, 20:24]), reads=["sm5"], writes=["sm5"])
                          gv = gates[:, g * 12 + br:g * 12 + br + 12:3]
                          TT("dve", sm[:, 20:24], sm[:, 20:24], gv, ALU.mult, ["sm5", "gates"], ["sm5"])
                          if br == 0:
                              TT("dve", og, t3[:, :, 0:64], bc_last(sm[:, 20:24], 64), ALU.mult, [pk(BK_T), "sm5"], ["onsa"])
                          else:
                              a = nscr()
                              TT("dve", v3(scr[a][:, 0:256], 4), t3[:, :, 0:64], bc_last(sm[:, 20:24], 64), ALU.mult, [pk(BK_T), "sm5"], [("scr", a)])
                              TT("pool", og, og, v3(scr[a][:, 0:256], 4), ALU.add, ["onsa", ("scr", a)], ["onsa"])

                      def run_branch(items, extra=None, lag=2):
                          n_ = len(items)
                          pend = []

                          def pv(idx, pi, it):
                              MM(ps[BK_O][0:65, :], it[4], PT[pi][:], idx == 0, idx == n_ - 1, [it[5], ("PT", pi)], [pk(BK_O)])
                              if extra is not None:
                                  extra(idx, pi, n_)

                          for idx, it in enumerate(items):
                              pi = score(it[0], it[1], it[2], it[3])
                              pend.append((idx, pi, it))
                              if len(pend) > lag:
                                  pv(*pend.pop(0))
                          while pend:
                              pv(*pend.pop(0))

                      ktl = j // 8
                      nkt = ktl + 1
                      items = []
                      for kt in range(nkt):
                          maskT, mk = None, None
                          if kt == ktl:
                              off = 120 - 16 * (j % 8)
                              maskT, mk = cbf["hc"][:, off:off + 128], "c_hc"
                          elif j % 8 == 0 and kt == ktl - 1:
                              maskT, mk = cbf["hprev"][:], "c_hprev"
                          items.append((KcT[:, kt * 128:(kt + 1) * 128], "KcT", maskT, mk,
                                        Vc[:, (g * 4 + kt) * 66:(g * 4 + kt) * 66 + 65], "Vc", kt))

                      def cmp_extra(idx, pi, n_):
                          for h in range(4):
                              bk = BK_U0 if h < 2 else BK_U1
                              MM(ps[bk][:, (h % 2) * 129:(h % 2) * 129 + 129], PT[pi][:, h * 128:(h + 1) * 128], cbf["ovm"][:, idx * 129:(idx + 1) * 129],
                                 idx == 0 and h % 2 == 0, idx == n_ - 1, [("PT", pi), "c_ovm"], [pk(bk)])

                      run_branch(items, cmp_extra)
                      combine(0)
                      CP("dve", us[:, 0:258], ps[BK_U0][:, 0:258], [pk(BK_U0)], ["us"])
                      CP("dve", us[:, 258:516], ps[BK_U1][:, 0:258], [pk(BK_U1)], ["us"])
                      us3 = v3(us[:], 4)
                      TS("dve", sm[:, 16:20].unsqueeze(2), us3[:, :, 128:129], 1e-30, None, ALU.max, None, ["us"], ["sm4"])
                      P.op("dve", lambda e: e.reciprocal(out=sm[:, 16:20], in_=sm[:, 16:20]), reads=["sm4"], writes=["sm4"])
                      g0 = 128 - 4 * j
                      STT("dve", acc[:], us3[:, 0, 0:128], sm[:, 16:17], cf["gsel"][:, g0:g0 + 128], ALU.mult, ALU.add, ["us", "sm4", "c_gsel"], ["acc"])
                      for h in range(1, 4):
                          STT("dve", acc[:], us3[:, h, 0:128], sm[:, 16 + h:17 + h], acc[:], ALU.mult, ALU.add, ["us", "sm4", "acc"], ["acc"])
                      MEMSET("dve", acc[:, 0:1], 3e9, ["acc"])
                      P.op("dve", lambda e: e.max(out=m8[:, 0:8], in_=acc[:]), reads=["acc"], writes=["m8a"])
                      P.op("dve", lambda e: e.match_replace(out=accw[:], in_to_replace=m8[:, 0:8], in_values=acc[:], imm_value=-3e38),
                           reads=["acc", "m8a"], writes=["accw"])
                      P.op("dve", lambda e: e.max(out=m8[:, 8:16], in_=accw[:]), reads=["accw"], writes=["m8b"])
                      TS("dve", accw[:], acc[:], m8[:, 15:16], None, ALU.is_lt, None, ["acc", "m8b"], ["accw"])
                      TS("dve", selb[:], accw[:], NEG, None, ALU.mult, None, ["accw"], ["selb"])
                      nblk = 4 * j + 4
                      for c_ in range(j // 2 + 1):
                          nb_ = min(8, nblk - 8 * c_)
                          CP("dve" if c_ % 3 != 2 else "pool", v3(selx[:, c_ * 512:c_ * 512 + nb_ * 64], nb_), bc_last(selb[:, 8 * c_:8 * c_ + nb_], 64),
                             ["selb"], [("gT", c_)])
                      TT("dve", selx[:, 2 * j * 128:(2 * j + 2) * 128], selx[:, 2 * j * 128:(2 * j + 2) * 128], cbf["cmT"][:], ALU.add,
                         [("gT", j // 2), "c_cmT"], [("gT", j // 2)])
                      if dbg and j == 1 and g == 0:
                          DBG(acc[:], ["acc"], 128)
                      wk = [kt for kt in range(2 * j - 4, 2 * j + 2) if kt >= 0]
                      items = []
                      for kt in wk:
                          w_ = kt - (2 * j - 4)
                          mi = {0: 0, 1: 1, 4: 2, 5: 3}.get(w_)
                          maskT, mk = (None, None) if mi is None else (cbf["wmT"][:, mi * 128:(mi + 1) * 128], "c_wmT")
                          items.append((Kwin[:, (kt % 8) * 128:(kt % 8) * 128 + 128], "Kwin", maskT, mk,
                                        Vwin[:, (kt % 8) * 132 + g * 66:(kt % 8) * 132 + g * 66 + 65], "Vwin", kt))
                      run_branch(items)
                      combine(2)
                      nk = 2 * j + 2
                      items = [(Ksel[:, kt * 128:(kt + 1) * 128], "Ksel", selx[:, kt * 128:(kt + 1) * 128], ("gT", kt // 4),
                                Vsel[:, kt * 132 + g * 66:kt * 132 + g * 66 + 65], "Vsel", kt) for kt in range(nk)]
                      run_branch(items)
                      combine(1)
                  if dbg and j == 1:
                      DBG(onsa[:], ["onsa"], 512)
                  B = nxt()
                  TR4(mixr, "mixr", B)
                  CP("act", mixTp[:, 512:1024], ps[B][:], [pk(B)], ["mixTp"])
                  CP("act", onsab[:], onsa[:], ["onsa"], ["onsab"])
                  B = nxt()
                  TR4(onsab, "onsab", B)
                  CP("act", mixTp[:, 0:512], ps[B][:], [pk(B)], ["mixTp"])
                  STOP(7)
                  for q4 in range(2):
                      Y = nxt()
                      for o4 in range(4):
                          oc = q4 * 4 + o4
                          s = load_w("woutr", (oc,), 1024)
                          slab = v3(wbuf[s][:, 0:1024], 8)
                          for kc in range(8):
                              MM(ps[Y][:, o4 * 128:(o4 + 1) * 128], slab[:, kc, :], mixTp[:, kc * 128:(kc + 1) * 128], kc == 0, kc == 7,
                                 [("wbuf", s), "mixTp"], [pk(Y)])
                      zv = v3(xo[:], 8)[:, q4 * 4:q4 * 4 + 4, oc0:oc0 + 128]
                      STT("dve", zv, v3(ps[Y][:], 4), 1.0 / ALPHA, zv, ALU.mult, ALU.add, [pk(Y), "xo"], ["xo"])
              STOP(8)
              if m % 2 == 1:
                  st = m // 2
                  LN(xo, xob, 1, "xo", "xob")
                  if dbg and st == 0:
                      DBG(xo[:], ["xo"], 4096)
                  FFN(1, xo, xob, "xo", "xob")
                  LN(xo, xob, 2, "xo", "xob")
                  DMA(v3(pTb[:], 2), pT_d[:, :, st * 512:(st + 1) * 512].rearrange("c p t -> p c t"), [], ["pTb"], queue="pool")
                  for oc in range(8):
                      s = load_w("wgater", (oc,), 1024)
                      slab = v3(wbuf[s][:, 0:1024], 8)
                      G_ = nxt()
                      for kc in range(8):
                          MM(ps[G_][:], slab[:, kc, :], xob[:, kc * 512:(kc + 1) * 512], kc == 0, kc == 7, [("wbuf", s), "xob"], [pk(G_)])
                      a = nscr()
                      ACT(scr[a][:], ps[G_][:], AF.Sigmoid, [pk(G_)], [("scr", a)])
                      s2 = load_w("wpler", (oc,), 256)
                      slab2 = v3(wbuf[s2][:, 0:256], 2)
                      E_ = nxt()
                      for kc in range(2):
                          MM(ps[E_][:], slab2[:, kc, :], pTb[:, kc * 512:(kc + 1) * 512], kc == 0, kc == 1, [("wbuf", s2), "pTb"], [pk(E_)])
                      TT("dve", scr[a][:], scr[a][:], ps[E_][:], ALU.mult, [("scr", a), pk(E_)], [("scr", a)])
                      zc = xo[:, oc * 512:(oc + 1) * 512]
                      STT("dve", zc, scr[a][:], 1.0 / ALPHA, zc, ALU.mult, ALU.add, [("scr", a), "xo"], ["xo"])
                  LN(xo, xob, 3, "xo", "xob")
                  DMA(out_d[:, :, st * 512:(st + 1) * 512].rearrange("c p t -> p c t"), v3(xo[:], 8), ["xo"], [("out", st)])

        try:
            main_loop()
        except _Stop:
            pass
        outs = [("out", st) for st in range(NB // 2)] + ([("dbg", s) for s in range(dbg_n[0])] if dbg else [])
        P.op("sp", None, reads=outs)
        P.emit(sems)
    return nc, P


_NC_CACHE = {}


def make_in_maps(inp, T):
    x = np.asarray(inp["x"], np.float32)
    p = np.asarray(inp["p"], np.float32)[0]
    B = x.shape[0]
    w = layout_weights({k: np.asarray(v, np.float32) for k, v in inp.items()})
    maps = []
    for core in range(2 * B):
        b, par = core // 2, core % 2
        d = dict(w)
        d.update(make_consts(T, par))
        d["xT"] = np.ascontiguousarray(x[b].T).reshape(8, 128, T)
        own = p[b].reshape(T // 256, 2, 128, 256)[:, par].reshape(T // 2, 256)
        d["pT"] = np.ascontiguousarray(own.T).reshape(2, 128, T // 2)
        maps.append(d)
    return maps


def assemble(results, B, T):
    out = np.zeros((B, T, D), np.float32)
    for core in range(2 * B):
        b, par = core // 2, core % 2
        o = results[core]["outT"].reshape(D, T // 2).T
        out[b].reshape(T // 256, 2, 128, D)[:, par] = o.reshape(T // 256, 128, D)
    return out


def kernel(**inputs):
    x = inputs["x"]
    B, T = x.shape[0], x.shape[1]
    key = T
    if key not in _NC_CACHE:
        _NC_CACHE[key] = build_nc(T)[0]
    nc = _NC_CACHE[key]
    maps = make_in_maps(inputs, T)
    res = run_bass_kernel_spmd(nc, maps, core_ids=list(range(2 * B)))
    return assemble(res.results, B, T)
```

```python
import os
from contextlib import ExitStack
import numpy as np
import concourse.bass as bass
import concourse.mybir as mybir
from concourse.bass_utils import run_bass_kernel_spmd

F32 = mybir.dt.float32
BF16 = mybir.dt.bfloat16
AF = mybir.ActivationFunctionType
ALU = mybir.AluOpType
AX = mybir.AxisListType

D = 1024
DFF = 2816
NJ = 22
ALPHA = 2.0 ** 0.25
LN_EPS = 1e-5
EPS_LN = LN_EPS / (ALPHA * ALPHA)
NEG = -30000.0


class Inst:
    __slots__ = ("eng", "fn", "deps", "needed", "is_dma", "sem", "val", "idx")

    def __init__(self, eng, fn, is_dma=False):
        self.eng = eng
        self.fn = fn
        self.deps = set()
        self.needed = False
        self.is_dma = is_dma
        self.sem = None
        self.val = 0
        self.idx = 0


class Prog:
    ENGS = ("pe", "act", "dve", "pool", "sp")

    def __init__(self, nc, dma_ring=12, same_engine_sync=True):
        self.nc = nc
        self.insts = []
        self.res = {}
        self.dma_ring = dma_ring
        self.same_engine_sync = same_engine_sync

    def _track(self, inst, reads, writes):
        res = self.res
        ps_reads = [k for k in reads if isinstance(k, tuple) and k[0] == "ps"]
        if ps_reads:
            reads = [k for k in reads if k not in ps_reads]
            writes = list(writes) + [k for k in ps_reads if k not in writes]
        for k in reads:
            r = res.get(k)
            if r is None:
                r = res[k] = [None, []]
            if r[0] is not None:
                inst.deps.add(r[0])
            r[1].append(inst)
        for k in writes:
            r = res.get(k)
            if r is None:
                r = res[k] = [None, []]
            if r[0] is not None:
                inst.deps.add(r[0])
            for q in r[1]:
                if q is not inst:
                    inst.deps.add(q)
            r[0] = inst
            r[1] = []
        inst.deps.discard(inst)
        inst.idx = len(self.insts)
        self.insts.append(inst)
        return inst

    def op(self, eng, fn, reads=(), writes=()):
        return self._track(Inst(eng, fn), reads, writes)

    def dma(self, fn, reads=(), writes=(), queue="sp"):
        return self._track(Inst(queue, fn, is_dma=True), reads, writes)

    def emit(self, sems):
        nc = self.nc
        ring_cnt = {}
        ring_last = {}
        for inst in self.insts:
            if inst.is_dma:
                q = inst.eng
                n = ring_cnt.get(q, 0)
                ring_cnt[q] = n + 1
                slot = n % self.dma_ring
                inst.sem = sems["ring_%s_%d" % (q, slot)]
                inst.val = 16 * (n // self.dma_ring + 1)
                prev = ring_last.get((q, slot))
                if prev is not None:
                    inst.deps.add(prev)
                ring_last[(q, slot)] = inst
        for inst in self.insts:
            for d in inst.deps:
                if d.is_dma:
                    d.needed = True
                elif d.eng == inst.eng and (d.eng == "pe" or not self.same_engine_sync):
                    pass
                else:
                    d.needed = True
        cnt = {e: 0 for e in self.ENGS}
        for inst in self.insts:
            if inst.is_dma:
                inst.needed = True
                continue
            if inst.needed:
                cnt[inst.eng] += 1
                inst.sem = sems[inst.eng]
                inst.val = cnt[inst.eng]
        lists = {e: [] for e in self.ENGS}
        known = {e: {} for e in self.ENGS}
        for inst in self.insts:
            waits = {}
            kn = known[inst.eng]
            for d in inst.deps:
                if not d.is_dma and d.eng == inst.eng and (d.eng == "pe" or not self.same_engine_sync):
                    continue
                key = id(d.sem)
                if kn.get(key, 0) >= d.val:
                    continue
                if key not in waits or waits[key][1] < d.val:
                    waits[key] = (d.sem, d.val)
            for key, (s, v) in waits.items():
                kn[key] = v
            lists[inst.eng].append((list(waits.values()), inst))
        self.stats = dict(n_inst=len(self.insts), per_eng={e: len(lists[e]) for e in self.ENGS})

        def run(e, items):
            for waits, inst in items:
                for (s, v) in waits:
                    e.wait_ge(s, v)
                if inst.fn is None:
                    continue
                r = inst.fn(e)
                if inst.needed:
                    r.then_inc(inst.sem, 16 if inst.is_dma else 1)

        with nc.Block() as block:
            @block.tensor
            def _(e):
                run(e, lists["pe"])

            @block.scalar
            def _(e):
                run(e, lists["act"])

            @block.vector
            def _(e):
                run(e, lists["dve"])

            @block.gpsimd
            def _(e):
                run(e, lists["pool"])

            @block.sync
            def _(e):
                run(e, lists["sp"])


def _gammas():
    return 1.0 - np.exp2(-5.0 - np.arange(4, dtype=np.float64))


def make_consts(T, par):
    c = {}
    c["identf"] = np.eye(128, dtype=np.float32)
    c["onesf"] = np.full((128, 128), 1.0 / D, np.float32)
    c["i4"] = np.tile(np.eye(128, dtype=np.float32), (1, 4))
    ov = np.zeros((128, 4, 129), np.float32)
    for kt in range(4):
        for p in range(128):
            cc = kt * 128 + p
            for s in range(128):
                lo = max(16 * cc, 64 * s)
                hi = min(16 * cc + 32, 64 * s + 64)
                if hi > lo:
                    ov[p, kt, s] = (hi - lo) / 32.0
            ov[p, kt, 128] = 1.0
    c["ovm"] = ov.reshape(128, 4 * 129)
    r = np.arange(128)
    x = np.arange(384)
    hw = np.where(16 * (x[None, :] - 8 * par - 120) + 31 <= r[:, None], 0.0, NEG)
    c["hc"] = hw.astype(np.float32)
    hp = np.zeros((128, 128), np.float32)
    if par == 0:
        hp[:15, 127] = NEG
    c["hprev"] = hp
    y = np.arange(384)
    curq = (r >= 64).astype(np.int64)
    rel = y[None, :] - 2 * par - 128
    g = np.zeros((128, 384), np.float32)
    g[rel > curq[:, None]] = -1e9
    g[rel == curq[:, None]] = 1e9
    g[rel == curq[:, None] - 1] = 2e9
    c["gsel"] = g
    caus = np.where(r[None, :] <= r[:, None], 0.0, NEG).astype(np.float32)
    allm = np.full((128, 128), NEG, np.float32)
    zero = np.zeros((128, 128), np.float32)
    c["cmT"] = np.concatenate([caus, allm] if par == 0 else [zero, caus], axis=1)
    upper = np.where(r[None, :] > r[:, None], 0.0, NEG).astype(np.float32)
    if par == 0:
        wm = [upper, zero, caus, allm]
    else:
        wm = [allm, upper, zero, caus]
    c["wmT"] = np.concatenate(wm, axis=1)
    gam = _gammas()
    i = np.arange(128, dtype=np.float64)
    tri = (r[:, None] <= r[None, :]).astype(np.float32)
    c["tri4"] = np.tile(tri, (1, 4))
    facq = np.stack([gam[h] ** (i + 1.0) for h in range(4)], 0)
    fack = np.stack([128.0 ** -0.5 * gam[h] ** (-(i + 1.0)) for h in range(4)], 0)
    c["facq"] = np.tile(facq.reshape(1, 512), (128, 1)).astype(np.float32)
    c["fack"] = np.tile(fack.reshape(1, 512), (128, 1)).astype(np.float32)
    fkv = np.stack([128.0 ** -0.5 * gam[h] ** (127.0 - i) for h in range(4)], 1)
    c["fkv"] = fkv.astype(np.float32)
    pos = np.arange(T, dtype=np.float32)
    freqs = (10000.0 ** (-np.arange(0, 128, 2, dtype=np.float32) / 128.0)).astype(np.float32)
    ang = pos[:, None] * freqs[None, :]
    cs, sn = np.cos(ang).astype(np.float32), np.sin(ang).astype(np.float32)
    rot = np.concatenate([cs, cs, -sn, sn], axis=1).reshape(T // 128, 128, 256)
    c["rotk"] = np.ascontiguousarray(rot)
    c["rotq"] = np.ascontiguousarray(rot[par::2])
    sel = np.zeros((128, 2), np.float32)
    sel[:, par] = 1.0
    c["selc"] = sel
    return c


def layout_weights(inp):
    w = {}
    w13 = inp["ffn_w13"][0]
    w2 = inp["ffn_w2"][0]
    a = w13[:, :, :DFF].reshape(2, 8, 128, NJ, 128)
    u = w13[:, :, DFF:].reshape(2, 8, 128, NJ, 128)
    au = np.stack([a, u], axis=4)
    w["w13r"] = np.ascontiguousarray(au.transpose(0, 3, 2, 1, 4, 5)).reshape(2, NJ, 128, 8 * 256)
    w["w2r"] = np.ascontiguousarray(w2.reshape(2, NJ, 128, 8, 128).transpose(0, 3, 2, 1, 4)).reshape(2, 8, 128, NJ * 128)
    win = inp["w_in"][0]
    qcols = [np.concatenate([np.arange((0 * 4 + r) * 64, (0 * 4 + r) * 64 + 64),
                             np.arange((4 + r) * 64, (4 + r) * 64 + 64)]) for r in range(4)]
    fcols = qcols + [np.arange(768, 896), np.arange(1024, 1152), np.arange(512, 640), np.arange(640, 768)]
    winF = np.stack([win[:, cc] for cc in fcols], 0)
    w["winF"] = np.ascontiguousarray(winF.reshape(8, 8, 128, 128).transpose(0, 2, 1, 3)).reshape(8, 128, 1024)
    pad = lambda cols: np.concatenate([win[:, cols], np.zeros((D, 512 - len(cols)), np.float32)], axis=1)
    tcols = [np.concatenate([np.arange(896, 1024), np.arange(1152, 1280)]),
             np.arange(1816, 2328), np.arange(2328, 2840),
             np.arange(1280, 1304), np.arange(1304, 1816), np.arange(2840, 3352)]
    winT = np.stack([pad(cc) for cc in tcols], 0)
    w["winT"] = np.ascontiguousarray(winT.reshape(6, 8, 128, 512).transpose(0, 2, 1, 3)).reshape(6, 128, 4096)
    rl = lambda m, kcn: np.ascontiguousarray(m.reshape(kcn, 128, 8, 128).transpose(2, 1, 0, 3)).reshape(8, 128, kcn * 128)
    w["woutr"] = rl(inp["w_out"][0], 8)
    w["wgater"] = rl(inp["w_ple_gate"][0], 8)
    w["wpler"] = rl(inp["w_ple"][0], 2)
    cw1 = inp["cmp_w1"][0].reshape(2, 32, 64, 2, 128)
    zz = np.zeros_like(cw1)
    cw1 = np.stack([np.concatenate([cw1, zz], axis=2), np.concatenate([zz, cw1], axis=2)], axis=1)
    w["cw1r"] = np.ascontiguousarray(cw1.transpose(0, 1, 4, 3, 2, 5)).reshape(2, 2, 2, 128, 32 * 128)
    cw2 = inp["cmp_w2"][0].reshape(2, 2, 128, 64)
    cw2 = np.concatenate([cw2, cw2], axis=3)
    w["cw2d"] = np.ascontiguousarray(cw2.transpose(2, 0, 1, 3)).reshape(128, 512)
    pos = inp["cmp_pos"][0]
    posT = np.concatenate([pos, pos], axis=2).transpose(2, 0, 1)
    w["posT"] = np.ascontiguousarray(posT).reshape(128, 64)
    lng = inp["ln_g"][0].reshape(4, 8, 128).transpose(2, 0, 1)
    lnb = inp["ln_b"][0].reshape(4, 8, 128).transpose(2, 0, 1)
    w["lnp"] = np.ascontiguousarray(np.stack([lng, lnb], 1)).reshape(128, 64)
    gn = np.stack([inp["ret_gn_g"][0], inp["ret_gn_b"][0]], 0).reshape(1, 1024)
    w["gnp"] = np.ascontiguousarray(np.tile(gn, (128, 1)))
    return w


class _Stop(Exception):
    pass


def build_nc(T, dbg=False, stop=0):
    NB = T // 512
    NT = T // 128
    TO = T // 2
    gam = _gammas()
    nc = bass.Bass("TRN2", target_bir_lowering=False)
    P = Prog(nc)
    din = {}

    def dram_in(name, shape):
        din[name] = nc.dram_tensor(name, list(shape), F32, kind="ExternalInput").ap()
        return din[name]

    xT_d = dram_in("xT", (8, 128, T))
    pT_d = dram_in("pT", (2, 128, TO))
    wshapes = dict(w13r=(2, NJ, 128, 2048), w2r=(2, 8, 128, NJ * 128), winF=(8, 128, 1024), winT=(6, 128, 4096),
                   woutr=(8, 128, 1024), wgater=(8, 128, 1024), wpler=(8, 128, 256), cw1r=(2, 2, 2, 128, 4096))
    wsrc = {k: dram_in(k, s) for k, s in wshapes.items()}
    wscr = {k: nc.dram_tensor(k + "_bf", list(s), BF16, kind="Internal").ap() for k, s in wshapes.items()}
    small_f32 = dict(lnp=64, gnp=1024, fack=512, facq=512, fkv=4, selc=2, gsel=384, identf=128, onesf=128)
    small_bf = dict(cw2d=512, posT=64, i4=512, ovm=516, hc=384, hprev=128, cmT=256, wmT=512, tri4=512)
    for k, n in list(small_f32.items()) + list(small_bf.items()):
        dram_in(k, (128, n))
    rotk_d = dram_in("rotk", (NT, 128, 256))
    rotq_d = dram_in("rotq", (NT // 2, 128, 256))
    out_d = nc.dram_tensor("outT", [8, 128, TO], F32, kind="ExternalOutput").ap()
    if dbg:
        dbg_d = nc.dram_tensor("dbg", [16, 128, 4096], F32, kind="ExternalOutput").ap()

    with ExitStack() as es:
        def sb(name, cols, dt=F32):
            return es.enter_context(nc.sbuf_tensor("sb_" + name, [128, cols], dt))

        sems = {}
        for e in Prog.ENGS:
            sems[e] = es.enter_context(nc.semaphore("s_" + e))
        for q in ("sp", "pool"):
            for i in range(P.dma_ring):
                sems["ring_%s_%d" % (q, i)] = es.enter_context(nc.semaphore("r_%s_%d" % (q, i)))
        ps = [es.enter_context(nc.psum_tensor("ps%d" % i, [128, 512], F32)) for i in range(8)]
        rr = [0]

        def nxt():
            rr[0] = (rr[0] + 1) % 4
            return rr[0]

        def pk(b):
            return ("ps", b)

        def MM(out, lhsT, rhs, start, stop, r, w):
            P.op("pe", lambda e: e.matmul(out, lhsT=lhsT, rhs=rhs, start=start, stop=stop, skip_group_check=True),
                 reads=r, writes=w)

        def TR(out, in_, ident, r, w):
            P.op("pe", lambda e: e.transpose(out=out, in_=in_, identity=ident), reads=r, writes=w)

        def ACT(out, in_, func, r, w, scale=1.0):
            P.op("act", lambda e: e.activation(out=out, in_=in_, func=func, scale=scale), reads=r, writes=w)

        def CP(eng, out, in_, r, w):
            if eng == "act":
                P.op("act", lambda e: e.copy(out=out, in_=in_), reads=r, writes=w)
            else:
                P.op(eng, lambda e: e.tensor_copy(out=out, in_=in_), reads=r, writes=w)

        def TT(eng, out, in0, in1, op, r, w):
            P.op(eng, lambda e: e.tensor_tensor(out=out, in0=in0, in1=in1, op=op), reads=r, writes=w)

        def TS(eng, out, in0, s1, s2, op0, op1, r, w):
            if op1 is None:
                P.op(eng, lambda e: e.tensor_scalar(out=out, in0=in0, scalar1=s1, scalar2=None, op0=op0), reads=r, writes=w)
            else:
                P.op(eng, lambda e: e.tensor_scalar(out=out, in0=in0, scalar1=s1, scalar2=s2, op0=op0, op1=op1), reads=r, writes=w)

        def STT(eng, out, in0, scalar, in1, op0, op1, r, w):
            P.op(eng, lambda e: e.scalar_tensor_tensor(out=out, in0=in0, scalar=scalar, in1=in1, op0=op0, op1=op1),
                 reads=r, writes=w)

        def MEMSET(eng, ap, val, w):
            P.op(eng, lambda e: e.memset(ap, val), writes=w)

        def DMA(out, in_, r, w, queue="sp"):
            P.dma(lambda e: e.dma_start(out=out, in_=in_), reads=r, writes=w, queue=queue)

        dbg_n = [0]

        def DBG(ap, r, cols):
            if not dbg:
                return
            s = dbg_n[0]
            dbg_n[0] += 1
            DMA(dbg_d[s, 0:ap.shape[0], 0:cols], ap, r, [("dbg", s)])
            return s

        def v3(ap, a):
            return ap.rearrange("p (a b) -> p a b", a=a)

        def bc_mid(ap, n):
            return ap.unsqueeze(1).to_broadcast([ap.shape[0], n, ap.shape[1]])

        def bc_last(ap, n):
            return ap.unsqueeze(2).to_broadcast([ap.shape[0], ap.shape[1], n])

        xT = sb("xT", 4096)
        xb = sb("xb", 4096, BF16)
        gT = sb("gT", NJ * 512, BF16)
        selx = gT
        scr = [sb("scr%d" % i, 512) for i in range(5)]
        sc = [0]

        def nscr():
            sc[0] = (sc[0] + 1) % 5
            return sc[0]

        NW = 4
        wbuf = [sb("wbuf%d" % i, 2048, BF16) for i in range(NW)]
        wc = [0]

        def wslot():
            wc[0] = (wc[0] + 1) % NW
            return wc[0]

        Ksel = sb("Ksel", T, BF16)
        Vsel = sb("Vsel", NT * 132, BF16)
        Kwin = sb("Kwin", 1024, BF16)
        Vwin = sb("Vwin", 8 * 132, BF16)
        KcT = sb("KcT", 512, BF16)
        VcT = sb("VcT", 512, BF16)
        Vc = sb("Vc", 2 * 4 * 66, BF16)
        KR = sb("KR", 528, BF16)
        VR = sb("VR", 528, BF16)
        xo = sb("xo", 4096)
        xob = sb("xob", 4096, BF16)
        mixTp = sb("mixTp", 1024, BF16)
        pTb = sb("pTb", 1024, BF16)
        krot = sb("krot", 2048, BF16)
        vrb = sb("vrb", 2048, BF16)
        vtil = sb("vtil", 2048, BF16)
        Sst = [sb("Sst%d" % i, 512) for i in range(3)]
        rk = [sb("rk%d" % i, 256) for i in range(2)]
        rq = sb("rq", 256)
        QA = [sb("QA%d" % i, 512, BF16) for i in range(2)]
        gates = sb("gates", 32)
        qrot = sb("qrot", 512, BF16)
        sgr = sb("sgr", 512)
        krown = sb("krown", 512, BF16)
        vrown = sb("vrown", 512, BF16)
        Sown = [sb("Sown%d" % i, 512, BF16) for i in range(2)]
        kTt = sb("kTt", 512, BF16)
        qTt = sb("qTt", 512, BF16)
        ATt = sb("ATt", 512, BF16)
        mixr = sb("mixr", 512, BF16)
        PT = [sb("PT%d" % i, 512, BF16) for i in range(3)]
        ptc = [0]
        obr = sb("obr", 512)
        us = sb("us", 516)
        acc = sb("acc", 128)
        accw = sb("accw", 128)
        m8 = sb("m8", 16)
        selb = sb("selb", 128, BF16)
        sm = sb("sm", 64)
        onsa = sb("onsa", 512)
        onsab = sb("onsab", 512, BF16)
        hx = sb("hx", 256)
        hgl = sb("hgl", 256, BF16)
        cb = sb("cb", 4)
        cf = {k: sb("c_" + k, n) for k, n in small_f32.items()}
        cbf = {k: sb("c_" + k, n, BF16) for k, n in small_bf.items()}
        identb = sb("identb", 128, BF16)
        onesb = sb("onesb", 128, BF16)
        lnbf = [PT[0], PT[1], PT[2], ATt]
        lnk = [("PT", 0), ("PT", 1), ("PT", 2), "ATt"]

        for k in small_f32:
            DMA(cf[k][:], din[k], [], ["c_" + k])
        for k in small_bf:
            DMA(cbf[k][:], din[k], [], ["c_" + k], queue="pool")
        DMA(identb[:], din["identf"], [], ["identb"], queue="pool")
        DMA(onesb[:], din["onesf"], [], ["onesb"], queue="pool")

        def cast_w(name, idx):
            src = wsrc[name]
            dst = wscr[name]
            for i in idx:
                src, dst = src[i], dst[i]
            DMA(dst, src, [], [(name,) + tuple(idx)], queue="pool")

        for j in range(NJ):
            cast_w("w13r", (0, j))
        for oc in range(8):
            cast_w("w2r", (0, oc))
        for fc in range(8):
            cast_w("winF", (fc,))
        for kv in range(2):
            for g in range(2):
                for hcn in range(2):
                    cast_w("cw1r", (kv, g, hcn))
        for s in range(6):
            cast_w("winT", (s,))
        for oc in range(8):
            cast_w("woutr", (oc,))
        for j in range(NJ):
            cast_w("w13r", (1, j))
        for oc in range(8):
            cast_w("w2r", (1, oc))
        for oc in range(8):
            cast_w("wgater", (oc,))
        for oc in range(8):
            cast_w("wpler", (oc,))

        def load_w(name, idx, cols, part=0, nparts=1, slot=None):
            s = wslot() if slot is None else slot
            src = wscr[name]
            for i in idx:
                src = src[i]
            pc = cols // nparts
            DMA(wbuf[s][:, 0:pc], src[:, part * pc:(part + 1) * pc], [(name,) + tuple(idx)], [("wbuf", s)])
            return s

        def load_parts(name, idx, cols, nparts):
            return [load_w(name, idx, cols, p, nparts) for p in range(nparts)]

        MEMSET("pool", KcT[:], 0.0, ["KcT"])
        MEMSET("pool", VcT[:], 0.0, ["VcT"])
        MEMSET("pool", Vc[:], 1.0, ["Vc"])
        MEMSET("pool", Vsel[:], 1.0, ["Vsel"])
        MEMSET("pool", Vwin[:], 1.0, ["Vwin"])
        MEMSET("pool", KR[:], 0.0, ["KR"])
        MEMSET("pool", VR[:], 0.0, ["VR"])
        MEMSET("pool", Sst[0][:], 0.0, [("Sst", 0)])
        MEMSET("pool", Kwin[:], 0.0, ["Kwin"])
        MEMSET("pool", QA[0][:], 0.0, [("QA", 0)])
        MEMSET("pool", QA[1][:], 0.0, [("QA", 1)])

        cbB = nxt()
        for kv in range(2):
            for hcn in range(2):
                pts = load_parts("cw1r", (kv, 0, hcn), 4096, 2)
                for l in range(32):
                    s = pts[l // 16]
                    MM(ps[cbB][:, kv * 2 + hcn:kv * 2 + hcn + 1], v3(wbuf[s][:, :], 16)[:, l % 16, :], cbf["posT"][:, kv * 32 + l:kv * 32 + l + 1],
                       l == 0, l == 31, [("wbuf", s), "c_posT"], [pk(cbB)])
        CP("dve", cb[:, 0:4], ps[cbB][:, 0:4], [pk(cbB)], ["cb"])

        lnp = cf["lnp"]

        def LN(z, zb, n, zk, zbk):
            MB, QB = nxt(), nxt()
            for c in range(8):
                s = (2 * c) % 4
                ACT(lnbf[s][:], z[:, c * 512:(c + 1) * 512], AF.Square, [zk], [lnk[s]])
                CP("act", lnbf[s + 1][:], z[:, c * 512:(c + 1) * 512], [zk], [lnk[s + 1]])
                MM(ps[MB][:], onesb[:], lnbf[s + 1][:], c == 0, c == 7, ["onesb", lnk[s + 1]], [pk(MB)])
                MM(ps[QB][:], onesb[:], lnbf[s][:], c == 0, c == 7, ["onesb", lnk[s]], [pk(QB)])
            sm_, s2_, sr_ = nscr(), nscr(), nscr()
            CP("act", scr[sm_][:], ps[MB][:], [pk(MB)], [("scr", sm_)])
            TT("pool", scr[s2_][:], scr[sm_][:], scr[sm_][:], ALU.mult, [("scr", sm_)], [("scr", s2_)])
            TT("dve", scr[sr_][:], ps[QB][:], scr[s2_][:], ALU.subtract, [pk(QB), ("scr", s2_)], [("scr", sr_)])
            TS("dve", scr[sr_][:], scr[sr_][:], EPS_LN, None, ALU.add, None, [("scr", sr_)], [("scr", sr_)])
            ACT(scr[sr_][:], scr[sr_][:], AF.Sqrt, [("scr", sr_)], [("scr", sr_)])
            P.op("dve", lambda e, sr_=sr_: e.reciprocal(out=scr[sr_][:], in_=scr[sr_][:]), reads=[("scr", sr_)], writes=[("scr", sr_)])
            for c in range(8):
                zc = z[:, c * 512:(c + 1) * 512]
                a = nscr()
                while a in (sm_, sr_):
                    a = nscr()
                TT("pool", scr[a][:], zc, scr[sm_][:], ALU.subtract, [zk, ("scr", sm_)], [("scr", a)])
                TT("dve", scr[a][:], scr[a][:], scr[sr_][:], ALU.mult, [("scr", a), ("scr", sr_)], [("scr", a)])
                TS("dve", zc, scr[a][:], lnp[:, n * 8 + c:n * 8 + c + 1], lnp[:, 32 + n * 8 + c:32 + n * 8 + c + 1],
                   ALU.mult, ALU.add, [("scr", a), "c_lnp"], [zk])
                CP("act", zb[:, c * 512:(c + 1) * 512], zc, [zk], [zbk])

        def FFN(f, z, zb, zk, zbk):
            for j in range(NJ):
                s = load_w("w13r", (f, j), 2048)
                slab = v3(wbuf[s][:, 0:2048], 8)
                A, U = nxt(), nxt()
                for kc in range(8):
                    MM(ps[A][:], slab[:, kc, 0:128], zb[:, kc * 512:(kc + 1) * 512], kc == 0, kc == 7, [("wbuf", s), zbk], [pk(A)])
                for kc in range(8):
                    MM(ps[U][:], slab[:, kc, 128:256], zb[:, kc * 512:(kc + 1) * 512], kc == 0, kc == 7, [("wbuf", s), zbk], [pk(U)])
                t = nscr()
                ACT(scr[t][:], ps[A][:], AF.Silu, [pk(A)], [("scr", t)])
                TT("dve", gT[:, j * 512:(j + 1) * 512], scr[t][:], ps[U][:], ALU.mult, [("scr", t), pk(U)], [("gT", j)])
            for oc in range(8):
                pts = load_parts("w2r", (f, oc), NJ * 128, 2)
                Y = nxt()
                for kc in range(NJ):
                    s = pts[kc // 11]
                    MM(ps[Y][:], v3(wbuf[s][:, 0:1408], 11)[:, kc % 11, :], gT[:, kc * 512:(kc + 1) * 512], kc == 0, kc == NJ - 1,
                       [("wbuf", s), ("gT", kc)], [pk(Y)])
                zc = z[:, oc * 512:(oc + 1) * 512]
                STT("dve", zc, ps[Y][:], 0.5 / ALPHA, zc, ALU.mult, ALU.add, [pk(Y), zk], [zk])

        def ROT(pb, tab, tabk, dst, dstk):
            a, b = nscr(), nscr()
            pv = v3(ps[pb][:], 4)
            TT("dve", v3(scr[a][:], 4), pv, bc_mid(tab[:, 0:128], 4), ALU.mult, [pk(pb), tabk], [("scr", a)])
            TT("dve", v3(scr[b][:], 4)[:, :, 0:64], pv[:, :, 64:128], bc_mid(tab[:, 128:192], 4), ALU.mult, [pk(pb), tabk], [("scr", b)])
            TT("dve", v3(scr[b][:], 4)[:, :, 64:128], pv[:, :, 0:64], bc_mid(tab[:, 192:256], 4), ALU.mult, [pk(pb), tabk], [("scr", b)])
            TT("pool", dst, scr[a][:], scr[b][:], ALU.add, [("scr", a), ("scr", b)], [dstk])

        def TR4(src, srck, bank):
            for c in range(4):
                MM(ps[bank][:, c * 128:(c + 1) * 128], src[:, c * 128:(c + 1) * 128], identb[:], True, True, [srck, "identb"], [pk(bank)])

        BK_O, BK_U0, BK_U1, BK_T = 7, 5, 6, 4

        def STOP(k):
            if stop == k:
                raise _Stop()

        def main_loop():
          for m in range(NB):
              if m == 0:
                  DMA(v3(xT[:], 8), xT_d[:, :, m * 512:(m + 1) * 512].rearrange("c p t -> p c t"), [], ["xT"])
              CP("act", xb[:, 0:2048], xT[:, 0:2048], ["xT"], ["xb"])
              CP("pool", xb[:, 2048:4096], xT[:, 2048:4096], ["xT"], ["xb"])
              FFN(0, xT, xb, "xT", "xb")
              LN(xT, xb, 0, "xT", "xb")
              if dbg and m == 0:
                  DBG(xT[:], ["xT"], 4096)
              for jj in range(2):
                  j = 2 * m + jj
                  oc0 = (j % 4) * 128
                  cA, cB = (2 * jj) * 128, (2 * jj + 1) * 128
                  xo3, xT3, xob3 = v3(xo[:], 8), v3(xT[:], 8), v3(xob[:], 8)
                  a = nscr()
                  TS("pool", v3(scr[a][:], 8)[:, :, 0:64], xT3[:, :, cA:cA + 64], cf["selc"][:, 0:1], None, ALU.mult, None, ["xT", "c_selc"], [("scr", a)])
                  b = nscr()
                  TS("pool", v3(scr[b][:], 8)[:, :, 0:64], xT3[:, :, cA + 64:cA + 128], cf["selc"][:, 0:1], None, ALU.mult, None, ["xT", "c_selc"], [("scr", b)])
                  STT("dve", xo3[:, :, oc0:oc0 + 64], xT3[:, :, cB:cB + 64], cf["selc"][:, 1:2], v3(scr[a][:], 8)[:, :, 0:64], ALU.mult, ALU.add,
                      ["xT", "c_selc", ("scr", a)], ["xo"])
                  STT("dve", xo3[:, :, oc0 + 64:oc0 + 128], xT3[:, :, cB + 64:cB + 128], cf["selc"][:, 1:2], v3(scr[b][:], 8)[:, :, 0:64], ALU.mult, ALU.add,
                      ["xT", "c_selc", ("scr", b)], ["xo"])
                  CP("act", xob3[:, :, oc0:oc0 + 128], xo3[:, :, oc0:oc0 + 128], ["xo"], ["xob"])
              if m + 1 < NB:
                  DMA(v3(xT[:], 8), xT_d[:, :, (m + 1) * 512:(m + 2) * 512].rearrange("c p t -> p c t"), [], ["xT"], queue="pool")
              STOP(1)
              for fc in (4, 5, 6, 7):
                  s = load_w("winF", (fc,), 1024)
                  slab = v3(wbuf[s][:, 0:1024], 8)
                  B = nxt()
                  for kc in range(8):
                      MM(ps[B][:], slab[:, kc, :], xb[:, kc * 512:(kc + 1) * 512], kc == 0, kc == 7, [("wbuf", s), "xb"], [pk(B)])
                  if fc == 4:
                      CP("act", Ksel[:, m * 512:(m + 1) * 512], ps[B][:], [pk(B)], ["Ksel"])
                  elif fc == 5:
                      CP("dve", Kwin[:, (m % 2) * 512:(m % 2) * 512 + 512], ps[B][:], [pk(B)], ["Kwin"])
                  else:
                      R_, rkey = (KR, "KR") if fc == 6 else (VR, "VR")
                      CP("pool", R_[:, 0:16], R_[:, 512:528], [rkey], [rkey])
                      CP("act" if fc == 6 else "dve", R_[:, 16:528], ps[B][:], [pk(B)], [rkey])
              STOP(2)
              H = BK_T
              cparts = []
              for kv in range(2):
                  for hcn in range(2):
                      for g in range(2):
                          for p_ in range(2):
                              def cpart(kv=kv, hcn=hcn, g=g, p_=p_, ci=len(cparts)):
                                  R_, rkey = (KR, "KR") if kv == 0 else (VR, "VR")
                                  s = load_w("cw1r", (kv, g, hcn), 4096, p_, 2, slot=2 + ci % 2)
                                  c0 = kv * 128 + g * 64 + hcn * 32
                                  for l in range(16 * p_, 16 * p_ + 16):
                                      MM(ps[H][:, c0:c0 + 32], v3(wbuf[s][:, :], 16)[:, l % 16, :], R_[:, l:l + 497:16],
                                         l == 0, l == 31, [("wbuf", s), rkey], [pk(H)])
                              cparts.append(cpart)
              STOP(3)
              for slab_i in range(3):
                  STOP(31 + slab_i)
                  pts = [load_w("winT", (slab_i,), 4096, 0, 2, slot=0), load_w("winT", (slab_i,), 4096, 1, 2, slot=1)]
                  for tt in range(4):
                      if cparts:
                          cparts.pop(0)()
                      n = 4 * m + tt
                      B = nxt()
                      ncol = 256 if slab_i == 0 else 512
                      for kc in range(8):
                          s = pts[kc // 4]
                          MM(ps[B][:, 0:ncol], xb[:, kc * 512 + tt * 128:kc * 512 + tt * 128 + 128], v3(wbuf[s][:, :], 4)[:, kc % 4, 0:ncol],
                             kc == 0, kc == 7, ["xb", ("wbuf", s)], [pk(B)])
                      if slab_i == 0 and os.environ.get("KSKIP") == "1":
                          pass
                      elif slab_i == 0:
                          for g in range(2):
                              if os.environ.get("KSKIP") != "2":
                                  CP("act", Vsel[:, n * 132 + g * 66:n * 132 + g * 66 + 64], ps[B][:, g * 64:(g + 1) * 64], [pk(B)], ["Vsel"])
                              if os.environ.get("KSKIP") == "3":
                                  continue
                              CP("dve", Vwin[:, (n % 8) * 132 + g * 66:(n % 8) * 132 + g * 66 + 64], ps[B][:, 128 + g * 64:128 + (g + 1) * 64],
                                 [pk(B)], ["Vwin"])
                      elif slab_i == 1:
                          DMA(rk[tt % 2][:], rotk_d[n], [], [("rk", tt % 2)])
                          ROT(B, rk[tt % 2], ("rk", tt % 2), krot[:, tt * 512:(tt + 1) * 512], ("krot", tt))
                      else:
                          CP("act", vrb[:, tt * 512:(tt + 1) * 512], ps[B][:], [pk(B)], [("vrb", tt)])
                          TT("dve", v3(vtil[:, tt * 512:(tt + 1) * 512], 4), v3(ps[B][:], 4), bc_last(cf["fkv"][:, 0:4], 128), ALU.mult,
                             [pk(B), "c_fkv"], [("vtil", tt)])
              STOP(34)
              for tt in range(4):
                  if cparts:
                      cparts.pop(0)()
                  n = 4 * m + tt
                  B = nxt()
                  for h in range(4):
                      MM(ps[B][:, h * 128:(h + 1) * 128], krot[:, tt * 512 + h * 128:tt * 512 + h * 128 + 128],
                         vtil[:, tt * 512 + h * 128:tt * 512 + h * 128 + 128], True, True, [("krot", tt), ("vtil", tt)], [pk(B)])
                  if tt % 2 == 1:
                      a = nscr()
                      TS("pool", scr[a][:], Sst[(n - 1) % 3][:], cf["selc"][:, 0:1], None, ALU.mult, None, [("Sst", (n - 1) % 3), "c_selc"], [("scr", a)])
                      STT("dve", Sown[tt // 2][:], Sst[n % 3][:], cf["selc"][:, 1:2], scr[a][:], ALU.mult, ALU.add,
                          [("Sst", n % 3), "c_selc", ("scr", a)], [("Sown", tt // 2)])
                  for h in range(4):
                      hs = slice(h * 128, (h + 1) * 128)
                      STT("dve", Sst[(n + 1) % 3][:, hs], Sst[n % 3][:, hs], float(gam[h] ** 128.0), ps[B][:, hs], ALU.mult, ALU.add,
                          [("Sst", n % 3), pk(B)], [("Sst", (n + 1) % 3)])
              while cparts:
                  cparts.pop(0)()
              STOP(21)
              for kv in range(2):
                  for hcn in range(2):
                      pv = ps[H][:, kv * 128:(kv + 1) * 128].rearrange("p (g h c) -> p g h c", g=2, h=2)[:, :, hcn, :]
                      hv = hx[:, kv * 128:(kv + 1) * 128].rearrange("p (g h c) -> p g h c", g=2, h=2)[:, :, hcn, :]
                      TS("dve", hv, pv, cb[:, kv * 2 + hcn:kv * 2 + hcn + 1], None, ALU.add, None, [pk(H), "cb"], ["hx"])
              STOP(22)
              a, b = nscr(), nscr()
              ACT(scr[a][:, 0:256], hx[:], AF.Square, ["hx"], [("scr", a)])
              TS("dve", scr[a][:, 0:256], scr[a][:, 0:256], 0.044715, 1.0, ALU.mult, ALU.add, [("scr", a)], [("scr", a)])
              TT("pool", scr[a][:, 0:256], scr[a][:, 0:256], hx[:], ALU.mult, [("scr", a), "hx"], [("scr", a)])
              ACT(scr[b][:, 0:256], scr[a][:, 0:256], AF.Sigmoid, [("scr", a)], [("scr", b)], scale=1.5957691216057308)
              TT("dve", hgl[:], scr[b][:, 0:256], hx[:], ALU.mult, [("scr", b), "hx"], ["hgl"])
              STOP(23)
              OB2 = nxt()
              for kv in range(2):
                  for g in range(2):
                      for hcn in range(2):
                          c0 = kv * 128 + g * 64 + hcn * 32
                          MM(ps[OB2][:, (kv * 2 + g) * 32:(kv * 2 + g) * 32 + 32], cbf["cw2d"][:, (kv * 2 + hcn) * 128:(kv * 2 + hcn) * 128 + 128],
                             hgl[:, c0:c0 + 32], hcn == 0, hcn == 1, ["c_cw2d", "hgl"], [pk(OB2)])
              for kv in range(2):
                  dstT, dk = (KcT, "KcT") if kv == 0 else (VcT, "VcT")
                  for g in range(2):
                      sk = 1 if m == 0 else 0
                      c_lo = 32 * m - 1 + sk
                      CP("dve" if g == 0 else "act", dstT[g * 64:(g + 1) * 64, c_lo:32 * m + 31],
                         ps[OB2][g * 64:(g + 1) * 64, (kv * 2 + g) * 32 + sk:(kv * 2 + g) * 32 + 32], [pk(OB2)], [dk])
              STOP(24)
              kts = [m // 4] + ([m // 4 - 1] if (m % 4 == 0 and m > 0) else [])
              for kt in kts:
                  TBk = nxt()
                  MM(ps[TBk][:, 0:128], VcT[:, kt * 128:(kt + 1) * 128], identb[:], True, True, ["VcT", "identb"], [pk(TBk)])
                  for g in range(2):
                      CP("dve", Vc[:, (g * 4 + kt) * 66:(g * 4 + kt) * 66 + 64], ps[TBk][:, g * 64:(g + 1) * 64], [pk(TBk)], ["Vc"])
              STOP(4)
              for jj in range(2):
                  j = 2 * m + jj
                  oc0 = (j % 4) * 128
                  cA, cB = (2 * jj) * 128, (2 * jj + 1) * 128
                  xo3, xT3, xob3 = v3(xo[:], 8), v3(xT[:], 8), v3(xob[:], 8)
                  QB = nxt()
                  for r_ in range(4):
                      s = load_w("winF", (r_,), 1024)
                      slab = v3(wbuf[s][:, 0:1024], 8)
                      for kc in range(8):
                          MM(ps[QB][:, r_ * 128:(r_ + 1) * 128], slab[:, kc, :], xob[:, kc * 512 + oc0:kc * 512 + oc0 + 128],
                             kc == 0, kc == 7, [("wbuf", s), "xob"], [pk(QB)])
                  CP("act", QA[0][0:64, :], ps[QB][0:64, :], [pk(QB)], [("QA", 0)])
                  CP("dve", QA[1][64:128, :], ps[QB][64:128, :], [pk(QB)], [("QA", 1)])
                  DMA(rq[:], rotq_d[j], [], ["rq"])
                  for slab_i in (3, 4, 5):
                      pts = load_parts("winT", (slab_i,), 4096, 2)
                      B = nxt()
                      ncol = 32 if slab_i == 3 else 512
                      for kc in range(8):
                          s = pts[kc // 4]
                          MM(ps[B][:, 0:ncol], xob[:, kc * 512 + oc0:kc * 512 + oc0 + 128], v3(wbuf[s][:, :], 4)[:, kc % 4, 0:ncol], kc == 0, kc == 7,
                             ["xob", ("wbuf", s)], [pk(B)])
                      if slab_i == 3:
                          ACT(gates[:], ps[B][:, 0:32], AF.Sigmoid, [pk(B)], ["gates"])
                      elif slab_i == 4:
                          ROT(B, rq, "rq", qrot[:], "qrot")
                      else:
                          ACT(sgr[:], ps[B][:], AF.Silu, [pk(B)], ["sgr"])
                  STOP(5)
                  tA, tB = 2 * jj, 2 * jj + 1
                  for (src, skey, dst, dkey) in ((krot, "krot", krown, "krown"), (vrb, "vrb", vrown, "vrown")):
                      a = nscr()
                      TS("pool", scr[a][:], src[:, tA * 512:(tA + 1) * 512], cf["selc"][:, 0:1], None, ALU.mult, None, [(skey, tA), "c_selc"], [("scr", a)])
                      STT("dve", dst[:], src[:, tB * 512:(tB + 1) * 512], cf["selc"][:, 1:2], scr[a][:], ALU.mult, ALU.add,
                          [(skey, tB), "c_selc", ("scr", a)], [dkey])
                  B = nxt()
                  TR4(krown, "krown", B)
                  TT("dve", kTt[:], ps[B][:], cf["fack"][:], ALU.mult, [pk(B), "c_fack"], ["kTt"])
                  B = nxt()
                  TR4(qrot, "qrot", B)
                  TT("dve", qTt[:], ps[B][:], cf["facq"][:], ALU.mult, [pk(B), "c_facq"], ["qTt"])
                  B = nxt()
                  for h in range(4):
                      hs = slice(h * 128, (h + 1) * 128)
                      MM(ps[B][:, hs], kTt[:, hs], qTt[:, hs], True, True, ["kTt", "qTt"], [pk(B)])
                  TT("dve", ATt[:], ps[B][:], cbf["tri4"][:], ALU.mult, [pk(B), "c_tri4"], ["ATt"])
                  B = nxt()
                  for h in range(4):
                      hs = slice(h * 128, (h + 1) * 128)
                      MM(ps[B][:, hs], ATt[:, hs], vrown[:, hs], True, False, ["ATt", "vrown"], [pk(B)])
                      MM(ps[B][:, hs], qTt[:, hs], Sown[jj][:, hs], False, True, ["qTt", ("Sown", jj)], [pk(B)])
                  o_, q_ = nscr(), nscr()
                  CP("act", scr[o_][:], ps[B][:], [pk(B)], [("scr", o_)])
                  ACT(scr[q_][:], ps[B][:], AF.Square, [pk(B)], [("scr", q_)])
                  P.op("dve", lambda e, o_=o_: e.reduce_sum(out=sm[:, 0:4], in_=v3(scr[o_][:], 4), axis=AX.X), reads=[("scr", o_)], writes=["sm0"])
                  P.op("dve", lambda e, q_=q_: e.reduce_sum(out=sm[:, 4:8], in_=v3(scr[q_][:], 4), axis=AX.X), reads=[("scr", q_)], writes=["sm1"])
                  TS("dve", sm[:, 0:4], sm[:, 0:4], 1.0 / 128.0, None, ALU.mult, None, ["sm0"], ["sm0"])
                  TT("dve", sm[:, 8:12], sm[:, 0:4], sm[:, 0:4], ALU.mult, ["sm0"], ["sm2"])
                  STT("dve", sm[:, 4:8], sm[:, 4:8], 1.0 / 128.0, sm[:, 8:12], ALU.mult, ALU.subtract, ["sm1", "sm2"], ["sm1"])
                  TS("dve", sm[:, 4:8], sm[:, 4:8], LN_EPS, None, ALU.add, None, ["sm1"], ["sm1"])
                  ACT(sm[:, 4:8], sm[:, 4:8], AF.Sqrt, ["sm1"], ["sm1"])
                  P.op("dve", lambda e: e.reciprocal(out=sm[:, 4:8], in_=sm[:, 4:8]), reads=["sm1"], writes=["sm1"])
                  TT("dve", v3(scr[o_][:], 4), v3(scr[o_][:], 4), bc_last(sm[:, 0:4], 128), ALU.subtract, [("scr", o_), "sm0"], [("scr", o_)])
                  TT("dve", v3(scr[o_][:], 4), v3(scr[o_][:], 4), bc_last(sm[:, 4:8], 128), ALU.mult, [("scr", o_), "sm1"], [("scr", o_)])
                  TT("pool", scr[o_][:], scr[o_][:], cf["gnp"][:, 0:512], ALU.mult, [("scr", o_), "c_gnp"], [("scr", o_)])
                  TT("pool", scr[o_][:], scr[o_][:], cf["gnp"][:, 512:1024], ALU.add, [("scr", o_), "c_gnp"], [("scr", o_)])
                  TT("dve", mixr[:], scr[o_][:], sgr[:], ALU.mult, [("scr", o_), "sgr"], ["mixr"])
                  if dbg and j == 0:
                      DBG(scr[o_][:], [("scr", o_)], 512)
                  STOP(6)
                  for g in range(2):
                      rows = slice(g * 64, (g + 1) * 64)

                      def score(lhsT, lk, maskT, mk):
                          S = nxt()
                          MM(ps[S][:], lhsT, QA[g][:], True, maskT is None, [lk, ("QA", g)], [pk(S)])
                          if maskT is not None:
                              MM(ps[S][:], maskT, cbf["i4"][:], False, True, [mk, "c_i4"], [pk(S)])
                          ptc[0] = (ptc[0] + 1) % 3
                          pi = ptc[0]
                          ACT(PT[pi][:], ps[S][:], AF.Exp, [pk(S)], [("PT", pi)], scale=0.125)
                          return pi

                      og = v3(onsa[:, g * 256:(g + 1) * 256], 4)

                      def combine(br):
                          CP("act", obr[0:65, :], ps[BK_O][0:65, :], [pk(BK_O)], ["obr"])
                          for h in range(4):
                              TR(ps[BK_T][:, h * 65:(h + 1) * 65], obr[0:65, h * 128:(h + 1) * 128], cf["identf"][0:65, 0:65],
                                 ["obr", "c_identf"], [pk(BK_T)])
                          t3 = v3(ps[BK_T][:, 0:260], 4)
                          smv = sm[:, 20:24].unsqueeze(2)
                          TS("dve", smv, t3[:, :, 64:65], 1e-30, None, ALU.max, None, [pk(BK_T)], ["sm5"])
                          P.op("dve", lambda e: e.reciprocal(out=sm[:, 20:24], in_=sm[:, 20:24]), reads=["sm5"], writes=["sm5"])
                          gv = gates[:, g * 12 + br:g * 12 + br + 12:3]
                          TT("dve", sm[:, 20:24], sm[:, 20:24], gv, ALU.mult, ["sm5", "gates"], ["sm5"])
                          if br == 0:
                              TT("dve", og, t3[:, :, 0:64], bc_last(sm[:, 20:24], 64), ALU.mult, [pk(BK_T), "sm5"], ["onsa"])
                          else:
                              a = nscr()
                              TT("dve", v3(scr[a][:, 0:256], 4), t3[:, :, 0:64], bc_last(sm[:, 20:24], 64), ALU.mult, [pk(BK_T), "sm5"], [("scr", a)])
                              TT("pool", og, og, v3(scr[a][:, 0:256], 4), ALU.add, ["onsa", ("scr", a)], ["onsa"])

                      def run_branch(items, extra=None, lag=2):
                          n_ = len(items)
                          pend = []

                          def pv(idx, pi, it):
                              MM(ps[BK_O][0:65, :], it[4], PT[pi][:], idx == 0, idx == n_ - 1, [it[5], ("PT", pi)], [pk(BK_O)])
                              if extra is not None:
                                  extra(idx, pi, n_)

                          for idx, it in enumerate(items):
                              pi = score(it[0], it[1], it[2], it[3])
                              pend.append((idx, pi, it))
                              if len(pend) > lag:
                                  pv(*pend.pop(0))
                          while pend:
                              pv(*pend.pop(0))

                      ktl = j // 8
                      nkt = ktl + 1
                      items = []
                      for kt in range(nkt):
                          maskT, mk = None, None
                          if kt == ktl:
                              off = 120 - 16 * (j % 8)
                              maskT, mk = cbf["hc"][:, off:off + 128], "c_hc"
                          elif j % 8 == 0 and kt == ktl - 1:
                              maskT, mk = cbf["hprev"][:], "c_hprev"
                          items.append((KcT[:, kt * 128:(kt + 1) * 128], "KcT", maskT, mk,
                                        Vc[:, (g * 4 + kt) * 66:(g * 4 + kt) * 66 + 65], "Vc", kt))

                      def cmp_extra(idx, pi, n_):
                          for h in range(4):
                              bk = BK_U0 if h < 2 else BK_U1
                              MM(ps[bk][:, (h % 2) * 129:(h % 2) * 129 + 129], PT[pi][:, h * 128:(h + 1) * 128], cbf["ovm"][:, idx * 129:(idx + 1) * 129],
                                 idx == 0 and h % 2 == 0, idx == n_ - 1, [("PT", pi), "c_ovm"], [pk(bk)])

                      run_branch(items, cmp_extra)
                      combine(0)
                      CP("dve", us[:, 0:258], ps[BK_U0][:, 0:258], [pk(BK_U0)], ["us"])
                      CP("dve", us[:, 258:516], ps[BK_U1][:, 0:258], [pk(BK_U1)], ["us"])
                      us3 = v3(us[:], 4)
                      TS("dve", sm[:, 16:20].unsqueeze(2), us3[:, :, 128:129], 1e-30, None, ALU.max, None, ["us"], ["sm4"])
                      P.op("dve", lambda e: e.reciprocal(out=sm[:, 16:20], in_=sm[:, 16:20]), reads=["sm4"], writes=["sm4"])
                      g0 = 128 - 4 * j
                      STT("dve", acc[:], us3[:, 0, 0:128], sm[:, 16:17], cf["gsel"][:, g0:g0 + 128], ALU.mult, ALU.add, ["us", "sm4", "c_gsel"], ["acc"])
                      for h in range(1, 4):
                          STT("dve", acc[:], us3[:, h, 0:128], sm[:, 16 + h:17 + h], acc[:], ALU.mult, ALU.add, ["us", "sm4", "acc"], ["acc"])
                      MEMSET("dve", acc[:, 0:1], 3e9, ["acc"])
                      P.op("dve", lambda e: e.max(out=m8[:, 0:8], in_=acc[:]), reads=["acc"], writes=["m8a"])
                      P.op("dve", lambda e: e.match_replace(out=accw[:], in_to_replace=m8[:, 0:8], in_values=acc[:], imm_value=-3e38),
                           reads=["acc", "m8a"], writes=["accw"])
                      P.op("dve", lambda e: e.max(out=m8[:, 8:16], in_=accw[:]), reads=["accw"], writes=["m8b"])
                      TS("dve", accw[:], acc[:], m8[:, 15:16], None, ALU.is_lt, None, ["acc", "m8b"], ["accw"])
                      TS("dve", selb[:], accw[:], NEG, None, ALU.mult, None, ["accw"], ["selb"])
                      nblk = 4 * j + 4
                      for c_ in range(j // 2 + 1):
                          nb_ = min(8, nblk - 8 * c_)
                          CP("dve" if c_ % 3 != 2 else "pool", v3(selx[:, c_ * 512:c_ * 512 + nb_ * 64], nb_), bc_last(selb[:, 8 * c_:8 * c_ + nb_], 64),
                             ["selb"], [("gT", c_)])
                      TT("dve", selx[:, 2 * j * 128:(2 * j + 2) * 128], selx[:, 2 * j * 128:(2 * j + 2) * 128], cbf["cmT"][:], ALU.add,
                         [("gT", j // 2), "c_cmT"], [("gT", j // 2)])
                      if dbg and j == 1 and g == 0:
                          DBG(acc[:], ["acc"], 128)
                      wk = [kt for kt in range(2 * j - 4, 2 * j + 2) if kt >= 0]
                      items = []
                      for kt in wk:
                          w_ = kt - (2 * j - 4)
                          mi = {0: 0, 1: 1, 4: 2, 5: 3}.get(w_)
                          maskT, mk = (None, None) if mi is None else (cbf["wmT"][:, mi * 128:(mi + 1) * 128], "c_wmT")
                          items.append((Kwin[:, (kt % 8) * 128:(kt % 8) * 128 + 128], "Kwin", maskT, mk,
                                        Vwin[:, (kt % 8) * 132 + g * 66:(kt % 8) * 132 + g * 66 + 65], "Vwin", kt))
                      run_branch(items)
                      combine(2)
                      nk = 2 * j + 2
                      items = [(Ksel[:, kt * 128:(kt + 1) * 128], "Ksel", selx[:, kt * 128:(kt + 1) * 128], ("gT", kt // 4),
                                Vsel[:, kt * 132 + g * 66:kt * 132 + g * 66 + 65], "Vsel", kt) for kt in range(nk)]
                      run_branch(items)
                      combine(1)
                  if dbg and j == 1:
                      DBG(onsa[:], ["onsa"], 512)
                  B = nxt()
                  TR4(mixr, "mixr", B)
                  CP("act", mixTp[:, 512:1024], ps[B][:], [pk(B)], ["mixTp"])
                  CP("act", onsab[:], onsa[:], ["onsa"], ["onsab"])
                  B = nxt()
                  TR4(onsab, "onsab", B)
                  CP("act", mixTp[:, 0:512], ps[B][:], [pk(B)], ["mixTp"])
                  STOP(7)
                  for q4 in range(2):
                      Y = nxt()
                      for o4 in range(4):
                          oc = q4 * 4 + o4
                          s = load_w("woutr", (oc,), 1024)
                          slab = v3(wbuf[s][:, 0:1024], 8)
                          for kc in range(8):
                              MM(ps[Y][:, o4 * 128:(o4 + 1) * 128], slab[:, kc, :], mixTp[:, kc * 128:(kc + 1) * 128], kc == 0, kc == 7,
                                 [("wbuf", s), "mixTp"], [pk(Y)])
                      zv = v3(xo[:], 8)[:, q4 * 4:q4 * 4 + 4, oc0:oc0 + 128]
                      STT("dve", zv, v3(ps[Y][:], 4), 1.0 / ALPHA, zv, ALU.mult, ALU.add, [pk(Y), "xo"], ["xo"])
              STOP(8)
              if m % 2 == 1:
                  st = m // 2
                  LN(xo, xob, 1, "xo", "xob")
                  if dbg and st == 0:
                      DBG(xo[:], ["xo"], 4096)
                  FFN(1, xo, xob, "xo", "xob")
                  LN(xo, xob, 2, "xo", "xob")
                  DMA(v3(pTb[:], 2), pT_d[:, :, st * 512:(st + 1) * 512].rearrange("c p t -> p c t"), [], ["pTb"], queue="pool")
                  for oc in range(8):
                      s = load_w("wgater", (oc,), 1024)
                      slab = v3(wbuf[s][:, 0:1024], 8)
                      G_ = nxt()
                      for kc in range(8):
                          MM(ps[G_][:], slab[:, kc, :], xob[:, kc * 512:(kc + 1) * 512], kc == 0, kc == 7, [("wbuf", s), "xob"], [pk(G_)])
                      a = nscr()
                      ACT(scr[a][:], ps[G_][:], AF.Sigmoid, [pk(G_)], [("scr", a)])
                      s2 = load_w("wpler", (oc,), 256)
                      slab2 = v3(wbuf[s2][:, 0:256], 2)
                      E_ = nxt()
                      for kc in range(2):
                          MM(ps[E_][:], slab2[:, kc, :], pTb[:, kc * 512:(kc + 1) * 512], kc == 0, kc == 1, [("wbuf", s2), "pTb"], [pk(E_)])
                      TT("dve", scr[a][:], scr[a][:], ps[E_][:], ALU.mult, [("scr", a), pk(E_)], [("scr", a)])
                      zc = xo[:, oc * 512:(oc + 1) * 512]
                      STT("dve", zc, scr[a][:], 1.0 / ALPHA, zc, ALU.mult, ALU.add, [("scr", a), "xo"], ["xo"])
                  LN(xo, xob, 3, "xo", "xob")
                  DMA(out_d[:, :, st * 512:(st + 1) * 512].rearrange("c p t -> p c t"), v3(xo[:], 8), ["xo"], [("out", st)], queue="pool")

        try:
            main_loop()
        except _Stop:
            pass
        outs = [("out", st) for st in range(NB // 2)] + ([("dbg", s) for s in range(dbg_n[0])] if dbg else [])
        P.op("sp", None, reads=outs)
        P.emit(sems)
    return nc, P


_NC_CACHE = {}


def make_in_maps(inp, T):
    x = np.asarray(inp["x"], np.float32)
    p = np.asarray(inp["p"], np.float32)[0]
    B = x.shape[0]
    w = layout_weights({k: np.asarray(v, np.float32) for k, v in inp.items()})
    maps = []
    for core in range(2 * B):
        b, par = core // 2, core % 2
        d = dict(w)
        d.update(make_consts(T, par))
        d["xT"] = np.ascontiguousarray(x[b].T).reshape(8, 128, T)
        own = p[b].reshape(T // 256, 2, 128, 256)[:, par].reshape(T // 2, 256)
        d["pT"] = np.ascontiguousarray(own.T).reshape(2, 128, T // 2)
        maps.append(d)
    return maps


def assemble(results, B, T):
    out = np.zeros((B, T, D), np.float32)
    for core in range(2 * B):
        b, par = core // 2, core % 2
        o = results[core]["outT"].reshape(D, T // 2).T
        out[b].reshape(T // 256, 2, 128, D)[:, par] = o.reshape(T // 256, 128, D)
    return out


def kernel(**inputs):
    x = inputs["x"]
    B, T = x.shape[0], x.shape[1]
    key = T
    if key not in _NC_CACHE:
        _NC_CACHE[key] = build_nc(T)[0]
    nc = _NC_CACHE[key]
    maps = make_in_maps(inputs, T)
    res = run_bass_kernel_spmd(nc, maps, core_ids=list(range(2 * B)))
    return assemble(res.results, B, T)
```

```python
import os
from contextlib import ExitStack
import numpy as np
import concourse.bass as bass
import concourse.mybir as mybir
from concourse.bass_utils import run_bass_kernel_spmd

F32 = mybir.dt.float32
BF16 = mybir.dt.bfloat16
AF = mybir.ActivationFunctionType
ALU = mybir.AluOpType
AX = mybir.AxisListType

D = 1024
DFF = 2816
NJ = 22
ALPHA = 2.0 ** 0.25
LN_EPS = 1e-5
EPS_LN = LN_EPS / (ALPHA * ALPHA)
NEG = -30000.0


class Inst:
    __slots__ = ("eng", "fn", "deps", "needed", "is_dma", "sem", "val", "idx")

    def __init__(self, eng, fn, is_dma=False):
        self.eng = eng
        self.fn = fn
        self.deps = set()
        self.needed = False
        self.is_dma = is_dma
        self.sem = None
        self.val = 0
        self.idx = 0


class Prog:
    ENGS = ("pe", "act", "dve", "pool", "sp")

    def __init__(self, nc, dma_ring=12, same_engine_sync=True):
        self.nc = nc
        self.insts = []
        self.res = {}
        self.dma_ring = dma_ring
        self.same_engine_sync = same_engine_sync

    def _track(self, inst, reads, writes):
        res = self.res
        ps_reads = [k for k in reads if isinstance(k, tuple) and k[0] == "ps"]
        if ps_reads:
            reads = [k for k in reads if k not in ps_reads]
            writes = list(writes) + [k for k in ps_reads if k not in writes]
        for k in reads:
            r = res.get(k)
            if r is None:
                r = res[k] = [None, []]
            if r[0] is not None:
                inst.deps.add(r[0])
            r[1].append(inst)
        for k in writes:
            r = res.get(k)
            if r is None:
                r = res[k] = [None, []]
            if r[0] is not None:
                inst.deps.add(r[0])
            for q in r[1]:
                if q is not inst:
                    inst.deps.add(q)
            r[0] = inst
            r[1] = []
        inst.deps.discard(inst)
        inst.idx = len(self.insts)
        self.insts.append(inst)
        return inst

    def op(self, eng, fn, reads=(), writes=()):
        return self._track(Inst(eng, fn), reads, writes)

    def dma(self, fn, reads=(), writes=(), queue="sp"):
        return self._track(Inst(queue, fn, is_dma=True), reads, writes)

    def emit(self, sems):
        nc = self.nc
        ring_cnt = {}
        ring_last = {}
        for inst in self.insts:
            if inst.is_dma:
                q = inst.eng
                n = ring_cnt.get(q, 0)
                ring_cnt[q] = n + 1
                slot = n % self.dma_ring
                inst.sem = sems["ring_%s_%d" % (q, slot)]
                inst.val = 16 * (n // self.dma_ring + 1)
                prev = ring_last.get((q, slot))
                if prev is not None:
                    inst.deps.add(prev)
                ring_last[(q, slot)] = inst
        for inst in self.insts:
            for d in inst.deps:
                if d.is_dma:
                    d.needed = True
                elif d.eng == inst.eng and (d.eng == "pe" or not self.same_engine_sync):
                    pass
                else:
                    d.needed = True
        cnt = {e: 0 for e in self.ENGS}
        for inst in self.insts:
            if inst.is_dma:
                inst.needed = True
                continue
            if inst.needed:
                cnt[inst.eng] += 1
                inst.sem = sems[inst.eng]
                inst.val = cnt[inst.eng]
        lists = {e: [] for e in self.ENGS}
        known = {e: {} for e in self.ENGS}
        for inst in self.insts:
            waits = {}
            kn = known[inst.eng]
            for d in inst.deps:
                if not d.is_dma and d.eng == inst.eng and (d.eng == "pe" or not self.same_engine_sync):
                    continue
                key = id(d.sem)
                if kn.get(key, 0) >= d.val:
                    continue
                if key not in waits or waits[key][1] < d.val:
                    waits[key] = (d.sem, d.val)
            for key, (s, v) in waits.items():
                kn[key] = v
            lists[inst.eng].append((list(waits.values()), inst))
        self.stats = dict(n_inst=len(self.insts), per_eng={e: len(lists[e]) for e in self.ENGS})

        def run(e, items):
            for waits, inst in items:
                for (s, v) in waits:
                    e.wait_ge(s, v)
                if inst.fn is None:
                    continue
                r = inst.fn(e)
                if inst.needed:
                    r.then_inc(inst.sem, 16 if inst.is_dma else 1)

        with nc.Block() as block:
            @block.tensor
            def _(e):
                run(e, lists["pe"])

            @block.scalar
            def _(e):
                run(e, lists["act"])

            @block.vector
            def _(e):
                run(e, lists["dve"])

            @block.gpsimd
            def _(e):
                run(e, lists["pool"])

            @block.sync
            def _(e):
                run(e, lists["sp"])


def _gammas():
    return 1.0 - np.exp2(-5.0 - np.arange(4, dtype=np.float64))


def make_consts(T, par):
    c = {}
    c["identf"] = np.eye(128, dtype=np.float32)
    c["onesf"] = np.full((128, 128), 1.0 / D, np.float32)
    c["i4"] = np.tile(np.eye(128, dtype=np.float32), (1, 4))
    ov = np.zeros((128, 4, 129), np.float32)
    for kt in range(4):
        for p in range(128):
            cc = kt * 128 + p
            for s in range(128):
                lo = max(16 * cc, 64 * s)
                hi = min(16 * cc + 32, 64 * s + 64)
                if hi > lo:
                    ov[p, kt, s] = (hi - lo) / 32.0
            ov[p, kt, 128] = 1.0
    c["ovm"] = ov.reshape(128, 4 * 129)
    r = np.arange(128)
    x = np.arange(384)
    hw = np.where(16 * (x[None, :] - 8 * par - 120) + 31 <= r[:, None], 0.0, NEG)
    c["hc"] = hw.astype(np.float32)
    hp = np.zeros((128, 128), np.float32)
    if par == 0:
        hp[:15, 127] = NEG
    c["hprev"] = hp
    y = np.arange(384)
    curq = (r >= 64).astype(np.int64)
    rel = y[None, :] - 2 * par - 128
    g = np.zeros((128, 384), np.float32)
    g[rel > curq[:, None]] = -1e9
    g[rel == curq[:, None]] = 1e9
    g[rel == curq[:, None] - 1] = 2e9
    c["gsel"] = g
    caus = np.where(r[None, :] <= r[:, None], 0.0, NEG).astype(np.float32)
    allm = np.full((128, 128), NEG, np.float32)
    zero = np.zeros((128, 128), np.float32)
    c["cmT"] = np.concatenate([caus, allm] if par == 0 else [zero, caus], axis=1)
    upper = np.where(r[None, :] > r[:, None], 0.0, NEG).astype(np.float32)
    if par == 0:
        wm = [upper, zero, caus, allm]
    else:
        wm = [allm, upper, zero, caus]
    c["wmT"] = np.concatenate(wm, axis=1)
    gam = _gammas()
    i = np.arange(128, dtype=np.float64)
    tri = (r[:, None] <= r[None, :]).astype(np.float32)
    c["tri4"] = np.tile(tri, (1, 4))
    facq = np.stack([gam[h] ** (i + 1.0) for h in range(4)], 0)
    fack = np.stack([128.0 ** -0.5 * gam[h] ** (-(i + 1.0)) for h in range(4)], 0)
    c["facq"] = np.tile(facq.reshape(1, 512), (128, 1)).astype(np.float32)
    c["fack"] = np.tile(fack.reshape(1, 512), (128, 1)).astype(np.float32)
    fkv = np.stack([128.0 ** -0.5 * gam[h] ** (127.0 - i) for h in range(4)], 1)
    c["fkv"] = fkv.astype(np.float32)
    pos = np.arange(T, dtype=np.float32)
    freqs = (10000.0 ** (-np.arange(0, 128, 2, dtype=np.float32) / 128.0)).astype(np.float32)
    ang = pos[:, None] * freqs[None, :]
    cs, sn = np.cos(ang).astype(np.float32), np.sin(ang).astype(np.float32)
    rot = np.concatenate([cs, cs, -sn, sn], axis=1).reshape(T // 128, 128, 256)
    c["rotk"] = np.ascontiguousarray(rot)
    c["rotq"] = np.ascontiguousarray(rot[par::2])
    sel = np.zeros((128, 2), np.float32)
    sel[:, par] = 1.0
    c["selc"] = sel
    return c


def layout_weights(inp):
    w = {}
    w13 = inp["ffn_w13"][0]
    w2 = inp["ffn_w2"][0]
    a = w13[:, :, :DFF].reshape(2, 8, 128, NJ, 128)
    u = w13[:, :, DFF:].reshape(2, 8, 128, NJ, 128)
    au = np.stack([a, u], axis=4)
    w["w13r"] = np.ascontiguousarray(au.transpose(0, 3, 2, 1, 4, 5)).reshape(2, NJ, 128, 8 * 256)
    w["w2r"] = np.ascontiguousarray(w2.reshape(2, NJ, 128, 8, 128).transpose(0, 3, 2, 1, 4)).reshape(2, 8, 128, NJ * 128)
    win = inp["w_in"][0]
    qcols = [np.concatenate([np.arange((0 * 4 + r) * 64, (0 * 4 + r) * 64 + 64),
                             np.arange((4 + r) * 64, (4 + r) * 64 + 64)]) for r in range(4)]
    fcols = qcols + [np.arange(768, 896), np.arange(1024, 1152), np.arange(512, 640), np.arange(640, 768)]
    winF = np.stack([win[:, cc] for cc in fcols], 0)
    w["winF"] = np.ascontiguousarray(winF.reshape(8, 8, 128, 128).transpose(0, 2, 1, 3)).reshape(8, 128, 1024)
    pad = lambda cols: np.concatenate([win[:, cols], np.zeros((D, 512 - len(cols)), np.float32)], axis=1)
    tcols = [np.concatenate([np.arange(896, 1024), np.arange(1152, 1280)]),
             np.arange(1816, 2328), np.arange(2328, 2840),
             np.arange(1280, 1304), np.arange(1304, 1816), np.arange(2840, 3352)]
    winT = np.stack([pad(cc) for cc in tcols], 0)
    w["winT"] = np.ascontiguousarray(winT.reshape(6, 8, 128, 512).transpose(0, 2, 1, 3)).reshape(6, 128, 4096)
    rl = lambda m, kcn: np.ascontiguousarray(m.reshape(kcn, 128, 8, 128).transpose(2, 1, 0, 3)).reshape(8, 128, kcn * 128)
    w["woutr"] = rl(inp["w_out"][0], 8)
    w["wgater"] = rl(inp["w_ple_gate"][0], 8)
    w["wpler"] = rl(inp["w_ple"][0], 2)
    cw1 = inp["cmp_w1"][0].reshape(2, 32, 64, 2, 128)
    zz = np.zeros_like(cw1)
    cw1 = np.stack([np.concatenate([cw1, zz], axis=2), np.concatenate([zz, cw1], axis=2)], axis=1)
    w["cw1r"] = np.ascontiguousarray(cw1.transpose(0, 1, 4, 3, 2, 5)).reshape(2, 2, 2, 128, 32 * 128)
    cw2 = inp["cmp_w2"][0].reshape(2, 2, 128, 64)
    cw2 = np.concatenate([cw2, cw2], axis=3)
    w["cw2d"] = np.ascontiguousarray(cw2.transpose(2, 0, 1, 3)).reshape(128, 512)
    pos = inp["cmp_pos"][0]
    posT = np.concatenate([pos, pos], axis=2).transpose(2, 0, 1)
    w["posT"] = np.ascontiguousarray(posT).reshape(128, 64)
    lng = inp["ln_g"][0].reshape(4, 8, 128).transpose(2, 0, 1)
    lnb = inp["ln_b"][0].reshape(4, 8, 128).transpose(2, 0, 1)
    w["lnp"] = np.ascontiguousarray(np.stack([lng, lnb], 1)).reshape(128, 64)
    gn = np.stack([inp["ret_gn_g"][0], inp["ret_gn_b"][0]], 0).reshape(1, 1024)
    w["gnp"] = np.ascontiguousarray(np.tile(gn, (128, 1)))
    return w


class _Stop(Exception):
    pass


def build_nc(T, dbg=False, stop=0):
    NB = T // 512
    NT = T // 128
    TO = T // 2
    gam = _gammas()
    nc = bass.Bass("TRN2", target_bir_lowering=False)
    P = Prog(nc)
    din = {}

    def dram_in(name, shape):
        din[name] = nc.dram_tensor(name, list(shape), F32, kind="ExternalInput").ap()
        return din[name]

    xT_d = dram_in("xT", (8, 128, T))
    pT_d = dram_in("pT", (2, 128, TO))
    wshapes = dict(w13r=(2, NJ, 128, 2048), w2r=(2, 8, 128, NJ * 128), winF=(8, 128, 1024), winT=(6, 128, 4096),
                   woutr=(8, 128, 1024), wgater=(8, 128, 1024), wpler=(8, 128, 256), cw1r=(2, 2, 2, 128, 4096))
    wsrc = {k: dram_in(k, s) for k, s in wshapes.items()}
    wscr = {k: nc.dram_tensor(k + "_bf", list(s), BF16, kind="Internal").ap() for k, s in wshapes.items()}
    small_f32 = dict(lnp=64, gnp=1024, fack=512, facq=512, fkv=4, selc=2, gsel=384, identf=128, onesf=128)
    small_bf = dict(cw2d=512, posT=64, i4=512, ovm=516, hc=384, hprev=128, cmT=256, wmT=512, tri4=512)
    for k, n in list(small_f32.items()) + list(small_bf.items()):
        dram_in(k, (128, n))
    rotk_d = dram_in("rotk", (NT, 128, 256))
    rotq_d = dram_in("rotq", (NT // 2, 128, 256))
    out_d = nc.dram_tensor("outT", [8, 128, TO], F32, kind="ExternalOutput").ap()
    if dbg:
        dbg_d = nc.dram_tensor("dbg", [16, 128, 4096], F32, kind="ExternalOutput").ap()

    with ExitStack() as es:
        def sb(name, cols, dt=F32):
            return es.enter_context(nc.sbuf_tensor("sb_" + name, [128, cols], dt))

        sems = {}
        for e in Prog.ENGS:
            sems[e] = es.enter_context(nc.semaphore("s_" + e))
        for q in ("sp", "pool"):
            for i in range(P.dma_ring):
                sems["ring_%s_%d" % (q, i)] = es.enter_context(nc.semaphore("r_%s_%d" % (q, i)))
        ps = [es.enter_context(nc.psum_tensor("ps%d" % i, [128, 512], F32)) for i in range(8)]
        rr = [0]

        def nxt():
            rr[0] = (rr[0] + 1) % 4
            return rr[0]

        def pk(b):
            return ("ps", b)

        def MM(out, lhsT, rhs, start, stop, r, w):
            P.op("pe", lambda e: e.matmul(out, lhsT=lhsT, rhs=rhs, start=start, stop=stop, skip_group_check=True),
                 reads=r, writes=w)

        def TR(out, in_, ident, r, w):
            P.op("pe", lambda e: e.transpose(out=out, in_=in_, identity=ident), reads=r, writes=w)

        def ACT(out, in_, func, r, w, scale=1.0):
            P.op("act", lambda e: e.activation(out=out, in_=in_, func=func, scale=scale), reads=r, writes=w)

        def CP(eng, out, in_, r, w):
            if eng == "act":
                P.op("act", lambda e: e.copy(out=out, in_=in_), reads=r, writes=w)
            else:
                P.op(eng, lambda e: e.tensor_copy(out=out, in_=in_), reads=r, writes=w)

        def TT(eng, out, in0, in1, op, r, w):
            P.op(eng, lambda e: e.tensor_tensor(out=out, in0=in0, in1=in1, op=op), reads=r, writes=w)

        def TS(eng, out, in0, s1, s2, op0, op1, r, w):
            if op1 is None:
                P.op(eng, lambda e: e.tensor_scalar(out=out, in0=in0, scalar1=s1, scalar2=None, op0=op0), reads=r, writes=w)
            else:
                P.op(eng, lambda e: e.tensor_scalar(out=out, in0=in0, scalar1=s1, scalar2=s2, op0=op0, op1=op1), reads=r, writes=w)

        def STT(eng, out, in0, scalar, in1, op0, op1, r, w):
            P.op(eng, lambda e: e.scalar_tensor_tensor(out=out, in0=in0, scalar=scalar, in1=in1, op0=op0, op1=op1),
                 reads=r, writes=w)

        def MEMSET(eng, ap, val, w):
            P.op(eng, lambda e: e.memset(ap, val), writes=w)

        def DMA(out, in_, r, w, queue="sp"):
            P.dma(lambda e: e.dma_start(out=out, in_=in_), reads=r, writes=w, queue=queue)

        dbg_n = [0]

        def DBG(ap, r, cols):
            if not dbg:
                return
            s = dbg_n[0]
            dbg_n[0] += 1
            DMA(dbg_d[s, 0:ap.shape[0], 0:cols], ap, r, [("dbg", s)])
            return s

        def v3(ap, a):
            return ap.rearrange("p (a b) -> p a b", a=a)

        def bc_mid(ap, n):
            return ap.unsqueeze(1).to_broadcast([ap.shape[0], n, ap.shape[1]])

        def bc_last(ap, n):
            return ap.unsqueeze(2).to_broadcast([ap.shape[0], ap.shape[1], n])

        xT = sb("xT", 4096)
        xb = sb("xb", 4096, BF16)
        gT = sb("gT", NJ * 512, BF16)
        selx = gT
        scr = [sb("scr%d" % i, 512) for i in range(5)]
        sc = [0]

        def nscr():
            sc[0] = (sc[0] + 1) % 5
            return sc[0]

        NW = 4
        wbuf = [sb("wbuf%d" % i, 2048, BF16) for i in range(NW)]
        wc = [0]

        def wslot():
            wc[0] = (wc[0] + 1) % NW
            return wc[0]

        Ksel = sb("Ksel", T, BF16)
        Vsel = sb("Vsel", NT * 132, BF16)
        Kwin = sb("Kwin", 1024, BF16)
        Vwin = sb("Vwin", 8 * 132, BF16)
        KcT = sb("KcT", 512, BF16)
        VcT = sb("VcT", 512, BF16)
        Vc = sb("Vc", 2 * 4 * 66, BF16)
        KR = sb("KR", 528, BF16)
        VR = sb("VR", 528, BF16)
        xo = sb("xo", 4096)
        xob = sb("xob", 4096, BF16)
        mixTp = sb("mixTp", 1024, BF16)
        pTb = sb("pTb", 1024, BF16)
        krot = sb("krot", 2048, BF16)
        vrb = sb("vrb", 2048, BF16)
        vtil = sb("vtil", 2048, BF16)
        Sst = [sb("Sst%d" % i, 512) for i in range(3)]
        rk = [sb("rk%d" % i, 256) for i in range(2)]
        rq = sb("rq", 256)
        QA = [sb("QA%d" % i, 512, BF16) for i in range(2)]
        gates = sb("gates", 32)
        qrot = sb("qrot", 512, BF16)
        sgr = sb("sgr", 512)
        krown = sb("krown", 512, BF16)
        vrown = sb("vrown", 512, BF16)
        Sown = [sb("Sown%d" % i, 512, BF16) for i in range(2)]
        kTt = sb("kTt", 512, BF16)
        qTt = sb("qTt", 512, BF16)
        ATt = sb("ATt", 512, BF16)
        mixr = sb("mixr", 512, BF16)
        PT = [sb("PT%d" % i, 512, BF16) for i in range(3)]
        ptc = [0]
        obr = sb("obr", 512)
        us = sb("us", 516)
        acc = sb("acc", 128)
        accw = sb("accw", 128)
        m8 = sb("m8", 16)
        selb = sb("selb", 128, BF16)
        sm = sb("sm", 64)
        onsa = sb("onsa", 512)
        onsab = sb("onsab", 512, BF16)
        hx = sb("hx", 256)
        hgl = sb("hgl", 256, BF16)
        cb = sb("cb", 4)
        cf = {k: sb("c_" + k, n) for k, n in small_f32.items()}
        cbf = {k: sb("c_" + k, n, BF16) for k, n in small_bf.items()}
        identb = sb("identb", 128, BF16)
        onesb = sb("onesb", 128, BF16)
        lnbf = [PT[0], PT[1], PT[2], ATt]
        lnk = [("PT", 0), ("PT", 1), ("PT", 2), "ATt"]

        for k in small_f32:
            DMA(cf[k][:], din[k], [], ["c_" + k])
        for k in small_bf:
            DMA(cbf[k][:], din[k], [], ["c_" + k], queue="pool")
        DMA(identb[:], din["identf"], [], ["identb"], queue="pool")
        DMA(onesb[:], din["onesf"], [], ["onesb"], queue="pool")

        def cast_w(name, idx):
            src = wsrc[name]
            dst = wscr[name]
            for i in idx:
                src, dst = src[i], dst[i]
            DMA(dst, src, [], [(name,) + tuple(idx)], queue="pool")

        for j in range(NJ):
            cast_w("w13r", (0, j))
        for oc in range(8):
            cast_w("w2r", (0, oc))
        for fc in range(8):
            cast_w("winF", (fc,))
        for kv in range(2):
            for g in range(2):
                for hcn in range(2):
                    cast_w("cw1r", (kv, g, hcn))
        for s in range(6):
            cast_w("winT", (s,))
        for oc in range(8):
            cast_w("woutr", (oc,))
        for j in range(NJ):
            cast_w("w13r", (1, j))
        for oc in range(8):
            cast_w("w2r", (1, oc))
        for oc in range(8):
            cast_w("wgater", (oc,))
        for oc in range(8):
            cast_w("wpler", (oc,))

        def load_w(name, idx, cols, part=0, nparts=1, slot=None):
            s = wslot() if slot is None else slot
            src = wscr[name]
            for i in idx:
                src = src[i]
            pc = cols // nparts
            DMA(wbuf[s][:, 0:pc], src[:, part * pc:(part + 1) * pc], [(name,) + tuple(idx)], [("wbuf", s)])
            return s

        def load_parts(name, idx, cols, nparts):
            return [load_w(name, idx, cols, p, nparts) for p in range(nparts)]

        MEMSET("pool", KcT[:], 0.0, ["KcT"])
        MEMSET("pool", VcT[:], 0.0, ["VcT"])
        MEMSET("pool", Vc[:], 1.0, ["Vc"])
        MEMSET("pool", Vsel[:], 1.0, ["Vsel"])
        MEMSET("pool", Vwin[:], 1.0, ["Vwin"])
        MEMSET("pool", KR[:], 0.0, ["KR"])
        MEMSET("pool", VR[:], 0.0, ["VR"])
        MEMSET("pool", Sst[0][:], 0.0, [("Sst", 0)])
        MEMSET("pool", Kwin[:], 0.0, ["Kwin"])
        MEMSET("pool", QA[0][:], 0.0, [("QA", 0)])
        MEMSET("pool", QA[1][:], 0.0, [("QA", 1)])

        cbB = nxt()
        for kv in range(2):
            for hcn in range(2):
                pts = load_parts("cw1r", (kv, 0, hcn), 4096, 2)
                for l in range(32):
                    s = pts[l // 16]
                    MM(ps[cbB][:, kv * 2 + hcn:kv * 2 + hcn + 1], v3(wbuf[s][:, :], 16)[:, l % 16, :], cbf["posT"][:, kv * 32 + l:kv * 32 + l + 1],
                       l == 0, l == 31, [("wbuf", s), "c_posT"], [pk(cbB)])
        CP("dve", cb[:, 0:4], ps[cbB][:, 0:4], [pk(cbB)], ["cb"])

        lnp = cf["lnp"]

        def LN(z, zb, n, zk, zbk):
            MB, QB = nxt(), nxt()
            for c in range(8):
                s = (2 * c) % 4
                ACT(lnbf[s][:], z[:, c * 512:(c + 1) * 512], AF.Square, [zk], [lnk[s]])
                CP("dve", lnbf[s + 1][:], z[:, c * 512:(c + 1) * 512], [zk], [lnk[s + 1]])
                MM(ps[MB][:], onesb[:], lnbf[s + 1][:], c == 0, c == 7, ["onesb", lnk[s + 1]], [pk(MB)])
                MM(ps[QB][:], onesb[:], lnbf[s][:], c == 0, c == 7, ["onesb", lnk[s]], [pk(QB)])
            sm_, s2_, sr_ = nscr(), nscr(), nscr()
            CP("act", scr[sm_][:], ps[MB][:], [pk(MB)], [("scr", sm_)])
            TT("pool", scr[s2_][:], scr[sm_][:], scr[sm_][:], ALU.mult, [("scr", sm_)], [("scr", s2_)])
            TT("dve", scr[sr_][:], ps[QB][:], scr[s2_][:], ALU.subtract, [pk(QB), ("scr", s2_)], [("scr", sr_)])
            TS("dve", scr[sr_][:], scr[sr_][:], EPS_LN, None, ALU.add, None, [("scr", sr_)], [("scr", sr_)])
            ACT(scr[sr_][:], scr[sr_][:], AF.Sqrt, [("scr", sr_)], [("scr", sr_)])
            P.op("dve", lambda e, sr_=sr_: e.reciprocal(out=scr[sr_][:], in_=scr[sr_][:]), reads=[("scr", sr_)], writes=[("scr", sr_)])
            for c in range(8):
                zc = z[:, c * 512:(c + 1) * 512]
                a = nscr()
                while a in (sm_, sr_):
                    a = nscr()
                TT("pool", scr[a][:], zc, scr[sm_][:], ALU.subtract, [zk, ("scr", sm_)], [("scr", a)])
                TT("dve", scr[a][:], scr[a][:], scr[sr_][:], ALU.mult, [("scr", a), ("scr", sr_)], [("scr", a)])
                TS("dve", zc, scr[a][:], lnp[:, n * 8 + c:n * 8 + c + 1], lnp[:, 32 + n * 8 + c:32 + n * 8 + c + 1],
                   ALU.mult, ALU.add, [("scr", a), "c_lnp"], [zk])
                CP("act", zb[:, c * 512:(c + 1) * 512], zc, [zk], [zbk])

        def FFN(f, z, zb, zk, zbk):
            for j in range(NJ):
                s = load_w("w13r", (f, j), 2048)
                slab = v3(wbuf[s][:, 0:2048], 8)
                A, U = nxt(), nxt()
                for kc in range(8):
                    MM(ps[A][:], slab[:, kc, 0:128], zb[:, kc * 512:(kc + 1) * 512], kc == 0, kc == 7, [("wbuf", s), zbk], [pk(A)])
                for kc in range(8):
                    MM(ps[U][:], slab[:, kc, 128:256], zb[:, kc * 512:(kc + 1) * 512], kc == 0, kc == 7, [("wbuf", s), zbk], [pk(U)])
                t = nscr()
                ACT(scr[t][:], ps[A][:], AF.Silu, [pk(A)], [("scr", t)])
                TT("dve", gT[:, j * 512:(j + 1) * 512], scr[t][:], ps[U][:], ALU.mult, [("scr", t), pk(U)], [("gT", j)])
            for oc in range(8):
                pts = load_parts("w2r", (f, oc), NJ * 128, 2)
                Y = nxt()
                for kc in range(NJ):
                    s = pts[kc // 11]
                    MM(ps[Y][:], v3(wbuf[s][:, 0:1408], 11)[:, kc % 11, :], gT[:, kc * 512:(kc + 1) * 512], kc == 0, kc == NJ - 1,
                       [("wbuf", s), ("gT", kc)], [pk(Y)])
                zc = z[:, oc * 512:(oc + 1) * 512]
                STT("dve", zc, ps[Y][:], 0.5 / ALPHA, zc, ALU.mult, ALU.add, [pk(Y), zk], [zk])

        def ROT(pb, tab, tabk, dst, dstk):
            a, b = nscr(), nscr()
            pv = v3(ps[pb][:], 4)
            TT("dve", v3(scr[a][:], 4), pv, bc_mid(tab[:, 0:128], 4), ALU.mult, [pk(pb), tabk], [("scr", a)])
            TT("dve", v3(scr[b][:], 4)[:, :, 0:64], pv[:, :, 64:128], bc_mid(tab[:, 128:192], 4), ALU.mult, [pk(pb), tabk], [("scr", b)])
            TT("dve", v3(scr[b][:], 4)[:, :, 64:128], pv[:, :, 0:64], bc_mid(tab[:, 192:256], 4), ALU.mult, [pk(pb), tabk], [("scr", b)])
            TT("pool", dst, scr[a][:], scr[b][:], ALU.add, [("scr", a), ("scr", b)], [dstk])

        def TR4(src, srck, bank):
            for c in range(4):
                MM(ps[bank][:, c * 128:(c + 1) * 128], src[:, c * 128:(c + 1) * 128], identb[:], True, True, [srck, "identb"], [pk(bank)])

        BK_O, BK_U0, BK_U1, BK_T = 7, 5, 6, 4

        def STOP(k):
            if stop == k:
                raise _Stop()

        def main_loop():
          for m in range(NB):
              if m == 0:
                  DMA(v3(xT[:], 8), xT_d[:, :, m * 512:(m + 1) * 512].rearrange("c p t -> p c t"), [], ["xT"])
              CP("act", xb[:, 0:2048], xT[:, 0:2048], ["xT"], ["xb"])
              CP("pool", xb[:, 2048:4096], xT[:, 2048:4096], ["xT"], ["xb"])
              FFN(0, xT, xb, "xT", "xb")
              LN(xT, xb, 0, "xT", "xb")
              if dbg and m == 0:
                  DBG(xT[:], ["xT"], 4096)
              for jj in range(2):
                  j = 2 * m + jj
                  oc0 = (j % 4) * 128
                  cA, cB = (2 * jj) * 128, (2 * jj + 1) * 128
                  xo3, xT3, xob3 = v3(xo[:], 8), v3(xT[:], 8), v3(xob[:], 8)
                  a = nscr()
                  TS("pool", v3(scr[a][:], 8)[:, :, 0:64], xT3[:, :, cA:cA + 64], cf["selc"][:, 0:1], None, ALU.mult, None, ["xT", "c_selc"], [("scr", a)])
                  b = nscr()
                  TS("pool", v3(scr[b][:], 8)[:, :, 0:64], xT3[:, :, cA + 64:cA + 128], cf["selc"][:, 0:1], None, ALU.mult, None, ["xT", "c_selc"], [("scr", b)])
                  STT("dve", xo3[:, :, oc0:oc0 + 64], xT3[:, :, cB:cB + 64], cf["selc"][:, 1:2], v3(scr[a][:], 8)[:, :, 0:64], ALU.mult, ALU.add,
                      ["xT", "c_selc", ("scr", a)], ["xo"])
                  STT("dve", xo3[:, :, oc0 + 64:oc0 + 128], xT3[:, :, cB + 64:cB + 128], cf["selc"][:, 1:2], v3(scr[b][:], 8)[:, :, 0:64], ALU.mult, ALU.add,
                      ["xT", "c_selc", ("scr", b)], ["xo"])
                  CP("act", xob3[:, :, oc0:oc0 + 128], xo3[:, :, oc0:oc0 + 128], ["xo"], ["xob"])
              if m + 1 < NB:
                  DMA(v3(xT[:], 8), xT_d[:, :, (m + 1) * 512:(m + 2) * 512].rearrange("c p t -> p c t"), [], ["xT"], queue="pool")
              STOP(1)
              for fc in (4, 5, 6, 7):
                  s = load_w("winF", (fc,), 1024)
                  slab = v3(wbuf[s][:, 0:1024], 8)
                  B = nxt()
                  for kc in range(8):
                      MM(ps[B][:], slab[:, kc, :], xb[:, kc * 512:(kc + 1) * 512], kc == 0, kc == 7, [("wbuf", s), "xb"], [pk(B)])
                  if fc == 4:
                      CP("act", Ksel[:, m * 512:(m + 1) * 512], ps[B][:], [pk(B)], ["Ksel"])
                  elif fc == 5:
                      CP("dve", Kwin[:, (m % 2) * 512:(m % 2) * 512 + 512], ps[B][:], [pk(B)], ["Kwin"])
                  else:
                      R_, rkey = (KR, "KR") if fc == 6 else (VR, "VR")
                      CP("pool", R_[:, 0:16], R_[:, 512:528], [rkey], [rkey])
                      CP("act" if fc == 6 else "dve", R_[:, 16:528], ps[B][:], [pk(B)], [rkey])
              STOP(2)
              H = BK_T
              cparts = []
              for kv in range(2):
                  for hcn in range(2):
                      for g in range(2):
                          for p_ in range(2):
                              def cpart(kv=kv, hcn=hcn, g=g, p_=p_, ci=len(cparts)):
                                  R_, rkey = (KR, "KR") if kv == 0 else (VR, "VR")
                                  s = load_w("cw1r", (kv, g, hcn), 4096, p_, 2, slot=2 + ci % 2)
                                  c0 = kv * 128 + g * 64 + hcn * 32
                                  for l in range(16 * p_, 16 * p_ + 16):
                                      MM(ps[H][:, c0:c0 + 32], v3(wbuf[s][:, :], 16)[:, l % 16, :], R_[:, l:l + 497:16],
                                         l == 0, l == 31, [("wbuf", s), rkey], [pk(H)])
                              cparts.append(cpart)
              STOP(3)
              for slab_i in range(3):
                  STOP(31 + slab_i)
                  pts = [load_w("winT", (slab_i,), 4096, 0, 2, slot=0), load_w("winT", (slab_i,), 4096, 1, 2, slot=1)]
                  for tt in range(4):
                      if cparts:
                          cparts.pop(0)()
                      n = 4 * m + tt
                      B = nxt()
                      ncol = 256 if slab_i == 0 else 512
                      for kc in range(8):
                          s = pts[kc // 4]
                          MM(ps[B][:, 0:ncol], xb[:, kc * 512 + tt * 128:kc * 512 + tt * 128 + 128], v3(wbuf[s][:, :], 4)[:, kc % 4, 0:ncol],
                             kc == 0, kc == 7, ["xb", ("wbuf", s)], [pk(B)])
                      if slab_i == 0 and os.environ.get("KSKIP") == "1":
                          pass
                      elif slab_i == 0:
                          for g in range(2):
                              if os.environ.get("KSKIP") != "2":
                                  CP("act", Vsel[:, n * 132 + g * 66:n * 132 + g * 66 + 64], ps[B][:, g * 64:(g + 1) * 64], [pk(B)], ["Vsel"])
                              if os.environ.get("KSKIP") == "3":
                                  continue
                              CP("dve", Vwin[:, (n % 8) * 132 + g * 66:(n % 8) * 132 + g * 66 + 64], ps[B][:, 128 + g * 64:128 + (g + 1) * 64],
                                 [pk(B)], ["Vwin"])
                      elif slab_i == 1:
                          DMA(rk[tt % 2][:], rotk_d[n], [], [("rk", tt % 2)])
                          ROT(B, rk[tt % 2], ("rk", tt % 2), krot[:, tt * 512:(tt + 1) * 512], ("krot", tt))
                      else:
                          CP("act", vrb[:, tt * 512:(tt + 1) * 512], ps[B][:], [pk(B)], [("vrb", tt)])
                          TT("dve", v3(vtil[:, tt * 512:(tt + 1) * 512], 4), v3(ps[B][:], 4), bc_last(cf["fkv"][:, 0:4], 128), ALU.mult,
                             [pk(B), "c_fkv"], [("vtil", tt)])
              STOP(34)
              for tt in range(4):
                  if cparts:
                      cparts.pop(0)()
                  n = 4 * m + tt
                  B = nxt()
                  for h in range(4):
                      MM(ps[B][:, h * 128:(h + 1) * 128], krot[:, tt * 512 + h * 128:tt * 512 + h * 128 + 128],
                         vtil[:, tt * 512 + h * 128:tt * 512 + h * 128 + 128], True, True, [("krot", tt), ("vtil", tt)], [pk(B)])
                  if tt % 2 == 1:
                      a = nscr()
                      TS("pool", scr[a][:], Sst[(n - 1) % 3][:], cf["selc"][:, 0:1], None, ALU.mult, None, [("Sst", (n - 1) % 3), "c_selc"], [("scr", a)])
                      STT("dve", Sown[tt // 2][:], Sst[n % 3][:], cf["selc"][:, 1:2], scr[a][:], ALU.mult, ALU.add,
                          [("Sst", n % 3), "c_selc", ("scr", a)], [("Sown", tt // 2)])
                  for h in range(4):
                      hs = slice(h * 128, (h + 1) * 128)
                      STT("dve", Sst[(n + 1) % 3][:, hs], Sst[n % 3][:, hs], float(gam[h] ** 128.0), ps[B][:, hs], ALU.mult, ALU.add,
                          [("Sst", n % 3), pk(B)], [("Sst", (n + 1) % 3)])
              while cparts:
                  cparts.pop(0)()
              STOP(21)
              for kv in range(2):
                  for hcn in range(2):
                      pv = ps[H][:, kv * 128:(kv + 1) * 128].rearrange("p (g h c) -> p g h c", g=2, h=2)[:, :, hcn, :]
                      hv = hx[:, kv * 128:(kv + 1) * 128].rearrange("p (g h c) -> p g h c", g=2, h=2)[:, :, hcn, :]
                      TS("dve", hv, pv, cb[:, kv * 2 + hcn:kv * 2 + hcn + 1], None, ALU.add, None, [pk(H), "cb"], ["hx"])
              STOP(22)
              a, b = nscr(), nscr()
              ACT(scr[a][:, 0:256], hx[:], AF.Square, ["hx"], [("scr", a)])
              TS("dve", scr[a][:, 0:256], scr[a][:, 0:256], 0.044715, 1.0, ALU.mult, ALU.add, [("scr", a)], [("scr", a)])
              TT("pool", scr[a][:, 0:256], scr[a][:, 0:256], hx[:], ALU.mult, [("scr", a), "hx"], [("scr", a)])
              ACT(scr[b][:, 0:256], scr[a][:, 0:256], AF.Sigmoid, [("scr", a)], [("scr", b)], scale=1.5957691216057308)
              TT("dve", hgl[:], scr[b][:, 0:256], hx[:], ALU.mult, [("scr", b), "hx"], ["hgl"])
              STOP(23)
              OB2 = nxt()
              for kv in range(2):
                  for g in range(2):
                      for hcn in range(2):
                          c0 = kv * 128 + g * 64 + hcn * 32
                          MM(ps[OB2][:, (kv * 2 + g) * 32:(kv * 2 + g) * 32 + 32], cbf["cw2d"][:, (kv * 2 + hcn) * 128:(kv * 2 + hcn) * 128 + 128],
                             hgl[:, c0:c0 + 32], hcn == 0, hcn == 1, ["c_cw2d", "hgl"], [pk(OB2)])
              for kv in range(2):
                  dstT, dk = (KcT, "KcT") if kv == 0 else (VcT, "VcT")
                  for g in range(2):
                      sk = 1 if m == 0 else 0
                      c_lo = 32 * m - 1 + sk
                      CP("dve" if g == 0 else "act", dstT[g * 64:(g + 1) * 64, c_lo:32 * m + 31],
                         ps[OB2][g * 64:(g + 1) * 64, (kv * 2 + g) * 32 + sk:(kv * 2 + g) * 32 + 32], [pk(OB2)], [dk])
              STOP(24)
              kts = [m // 4] + ([m // 4 - 1] if (m % 4 == 0 and m > 0) else [])
              for kt in kts:
                  TBk = nxt()
                  MM(ps[TBk][:, 0:128], VcT[:, kt * 128:(kt + 1) * 128], identb[:], True, True, ["VcT", "identb"], [pk(TBk)])
                  for g in range(2):
                      CP("dve", Vc[:, (g * 4 + kt) * 66:(g * 4 + kt) * 66 + 64], ps[TBk][:, g * 64:(g + 1) * 64], [pk(TBk)], ["Vc"])
              STOP(4)
              for jj in range(2):
                  j = 2 * m + jj
                  oc0 = (j % 4) * 128
                  cA, cB = (2 * jj) * 128, (2 * jj + 1) * 128
                  xo3, xT3, xob3 = v3(xo[:], 8), v3(xT[:], 8), v3(xob[:], 8)
                  QB = nxt()
                  for r_ in range(4):
                      s = load_w("winF", (r_,), 1024)
                      slab = v3(wbuf[s][:, 0:1024], 8)
                      for kc in range(8):
                          MM(ps[QB][:, r_ * 128:(r_ + 1) * 128], slab[:, kc, :], xob[:, kc * 512 + oc0:kc * 512 + oc0 + 128],
                             kc == 0, kc == 7, [("wbuf", s), "xob"], [pk(QB)])
                  CP("act", QA[0][0:64, :], ps[QB][0:64, :], [pk(QB)], [("QA", 0)])
                  CP("dve", QA[1][64:128, :], ps[QB][64:128, :], [pk(QB)], [("QA", 1)])
                  DMA(rq[:], rotq_d[j], [], ["rq"])
                  for slab_i in (3, 4, 5):
                      pts = load_parts("winT", (slab_i,), 4096, 2)
                      B = nxt()
                      ncol = 32 if slab_i == 3 else 512
                      for kc in range(8):
                          s = pts[kc // 4]
                          MM(ps[B][:, 0:ncol], xob[:, kc * 512 + oc0:kc * 512 + oc0 + 128], v3(wbuf[s][:, :], 4)[:, kc % 4, 0:ncol], kc == 0, kc == 7,
                             ["xob", ("wbuf", s)], [pk(B)])
                      if slab_i == 3:
                          ACT(gates[:], ps[B][:, 0:32], AF.Sigmoid, [pk(B)], ["gates"])
                      elif slab_i == 4:
                          ROT(B, rq, "rq", qrot[:], "qrot")
                      else:
                          ACT(sgr[:], ps[B][:], AF.Silu, [pk(B)], ["sgr"])
                  STOP(5)
                  tA, tB = 2 * jj, 2 * jj + 1
                  for (src, skey, dst, dkey) in ((krot, "krot", krown, "krown"), (vrb, "vrb", vrown, "vrown")):
                      a = nscr()
                      TS("pool", scr[a][:], src[:, tA * 512:(tA + 1) * 512], cf["selc"][:, 0:1], None, ALU.mult, None, [(skey, tA), "c_selc"], [("scr", a)])
                      STT("dve", dst[:], src[:, tB * 512:(tB + 1) * 512], cf["selc"][:, 1:2], scr[a][:], ALU.mult, ALU.add,
                          [(skey, tB), "c_selc", ("scr", a)], [dkey])
                  B = nxt()
                  TR4(krown, "krown", B)
                  TT("dve", kTt[:], ps[B][:], cf["fack"][:], ALU.mult, [pk(B), "c_fack"], ["kTt"])
                  B = nxt()
                  TR4(qrot, "qrot", B)
                  TT("dve", qTt[:], ps[B][:], cf["facq"][:], ALU.mult, [pk(B), "c_facq"], ["qTt"])
                  B = nxt()
                  for h in range(4):
                      hs = slice(h * 128, (h + 1) * 128)
                      MM(ps[B][:, hs], kTt[:, hs], qTt[:, hs], True, True, ["kTt", "qTt"], [pk(B)])
                  TT("dve", ATt[:], ps[B][:], cbf["tri4"][:], ALU.mult, [pk(B), "c_tri4"], ["ATt"])
                  B = nxt()
                  for h in range(4):
                      hs = slice(h * 128, (h + 1) * 128)
                      MM(ps[B][:, hs], ATt[:, hs], vrown[:, hs], True, False, ["ATt", "vrown"], [pk(B)])
                      MM(ps[B][:, hs], qTt[:, hs], Sown[jj][:, hs], False, True, ["qTt", ("Sown", jj)], [pk(B)])
                  o_, q_ = nscr(), nscr()
                  CP("act", scr[o_][:], ps[B][:], [pk(B)], [("scr", o_)])
                  ACT(scr[q_][:], ps[B][:], AF.Square, [pk(B)], [("scr", q_)])
                  P.op("dve", lambda e, o_=o_: e.reduce_sum(out=sm[:, 0:4], in_=v3(scr[o_][:], 4), axis=AX.X), reads=[("scr", o_)], writes=["sm0"])
                  P.op("dve", lambda e, q_=q_: e.reduce_sum(out=sm[:, 4:8], in_=v3(scr[q_][:], 4), axis=AX.X), reads=[("scr", q_)], writes=["sm1"])
                  TS("dve", sm[:, 0:4], sm[:, 0:4], 1.0 / 128.0, None, ALU.mult, None, ["sm0"], ["sm0"])
                  TT("dve", sm[:, 8:12], sm[:, 0:4], sm[:, 0:4], ALU.mult, ["sm0"], ["sm2"])
                  STT("dve", sm[:, 4:8], sm[:, 4:8], 1.0 / 128.0, sm[:, 8:12], ALU.mult, ALU.subtract, ["sm1", "sm2"], ["sm1"])
                  TS("dve", sm[:, 4:8], sm[:, 4:8], LN_EPS, None, ALU.add, None, ["sm1"], ["sm1"])
                  ACT(sm[:, 4:8], sm[:, 4:8], AF.Sqrt, ["sm1"], ["sm1"])
                  P.op("dve", lambda e: e.reciprocal(out=sm[:, 4:8], in_=sm[:, 4:8]), reads=["sm1"], writes=["sm1"])
                  TT("dve", v3(scr[o_][:], 4), v3(scr[o_][:], 4), bc_last(sm[:, 0:4], 128), ALU.subtract, [("scr", o_), "sm0"], [("scr", o_)])
                  TT("dve", v3(scr[o_][:], 4), v3(scr[o_][:], 4), bc_last(sm[:, 4:8], 128), ALU.mult, [("scr", o_), "sm1"], [("scr", o_)])
                  TT("pool", scr[o_][:], scr[o_][:], cf["gnp"][:, 0:512], ALU.mult, [("scr", o_), "c_gnp"], [("scr", o_)])
                  TT("pool", scr[o_][:], scr[o_][:], cf["gnp"][:, 512:1024], ALU.add, [("scr", o_), "c_gnp"], [("scr", o_)])
                  TT("dve", mixr[:], scr[o_][:], sgr[:], ALU.mult, [("scr", o_), "sgr"], ["mixr"])
                  if dbg and j == 0:
                      DBG(scr[o_][:], [("scr", o_)], 512)
                  STOP(6)
                  for g in range(2):
                      rows = slice(g * 64, (g + 1) * 64)

                      def score(lhsT, lk, maskT, mk):
                          S = nxt()
                          MM(ps[S][:], lhsT, QA[g][:], True, maskT is None, [lk, ("QA", g)], [pk(S)])
                          if maskT is not None:
                              MM(ps[S][:], maskT, cbf["i4"][:], False, True, [mk, "c_i4"], [pk(S)])
                          ptc[0] = (ptc[0] + 1) % 3
                          pi = ptc[0]
                          ACT(PT[pi][:], ps[S][:], AF.Exp, [pk(S)], [("PT", pi)], scale=0.125)
                          return pi

                      og = v3(onsa[:, g * 256:(g + 1) * 256], 4)

                      def combine(br):
                          CP("act", obr[0:65, :], ps[BK_O][0:65, :], [pk(BK_O)], ["obr"])
                          for h in range(4):
                              TR(ps[BK_T][:, h * 65:(h + 1) * 65], obr[0:65, h * 128:(h + 1) * 128], cf["identf"][0:65, 0:65],
                                 ["obr", "c_identf"], [pk(BK_T)])
                          t3 = v3(ps[BK_T][:, 0:260], 4)
                          smv = sm[:, 20:24].unsqueeze(2)
                          TS("dve", smv, t3[:, :, 64:65], 1e-30, None, ALU.max, None, [pk(BK_T)], ["sm5"])
                          P.op("dve", lambda e: e.reciprocal(out=sm[:, 20:24], in_=sm[:, 20:24]), reads=["sm5"], writes=["sm5"])
                          gv = gates[:, g * 12 + br:g * 12 + br + 12:3]
                          TT("dve", sm[:, 20:24], sm[:, 20:24], gv, ALU.mult, ["sm5", "gates"], ["sm5"])
                          if br == 0:
                              TT("dve", og, t3[:, :, 0:64], bc_last(sm[:, 20:24], 64), ALU.mult, [pk(BK_T), "sm5"], ["onsa"])
                          else:
                              a = nscr()
                              TT("dve", v3(scr[a][:, 0:256], 4), t3[:, :, 0:64], bc_last(sm[:, 20:24], 64), ALU.mult, [pk(BK_T), "sm5"], [("scr", a)])
                              TT("pool", og, og, v3(scr[a][:, 0:256], 4), ALU.add, ["onsa", ("scr", a)], ["onsa"])

                      def run_branch(items, extra=None, lag=2):
                          n_ = len(items)
                          pend = []

                          def pv(idx, pi, it):
                              MM(ps[BK_O][0:65, :], it[4], PT[pi][:], idx == 0, idx == n_ - 1, [it[5], ("PT", pi)], [pk(BK_O)])
                              if extra is not None:
                                  extra(idx, pi, n_)

                          for idx, it in enumerate(items):
                              pi = score(it[0], it[1], it[2], it[3])
                              pend.append((idx, pi, it))
                              if len(pend) > lag:
                                  pv(*pend.pop(0))
                          while pend:
                              pv(*pend.pop(0))

                      ktl = j // 8
                      nkt = ktl + 1
                      items = []
                      for kt in range(nkt):
                          maskT, mk = None, None
                          if kt == ktl:
                              off = 120 - 16 * (j % 8)
                              maskT, mk = cbf["hc"][:, off:off + 128], "c_hc"
                          elif j % 8 == 0 and kt == ktl - 1:
                              maskT, mk = cbf["hprev"][:], "c_hprev"
                          items.append((KcT[:, kt * 128:(kt + 1) * 128], "KcT", maskT, mk,
                                        Vc[:, (g * 4 + kt) * 66:(g * 4 + kt) * 66 + 65], "Vc", kt))

                      def cmp_extra(idx, pi, n_):
                          for h in range(4):
                              bk = BK_U0 if h < 2 else BK_U1
                              MM(ps[bk][:, (h % 2) * 129:(h % 2) * 129 + 129], PT[pi][:, h * 128:(h + 1) * 128], cbf["ovm"][:, idx * 129:(idx + 1) * 129],
                                 idx == 0 and h % 2 == 0, idx == n_ - 1, [("PT", pi), "c_ovm"], [pk(bk)])

                      run_branch(items, cmp_extra)
                      combine(0)
                      CP("dve", us[:, 0:258], ps[BK_U0][:, 0:258], [pk(BK_U0)], ["us"])
                      CP("dve", us[:, 258:516], ps[BK_U1][:, 0:258], [pk(BK_U1)], ["us"])
                      us3 = v3(us[:], 4)
                      TS("dve", sm[:, 16:20].unsqueeze(2), us3[:, :, 128:129], 1e-30, None, ALU.max, None, ["us"], ["sm4"])
                      P.op("dve", lambda e: e.reciprocal(out=sm[:, 16:20], in_=sm[:, 16:20]), reads=["sm4"], writes=["sm4"])
                      g0 = 128 - 4 * j
                      STT("dve", acc[:], us3[:, 0, 0:128], sm[:, 16:17], cf["gsel"][:, g0:g0 + 128], ALU.mult, ALU.add, ["us", "sm4", "c_gsel"], ["acc"])
                      for h in range(1, 4):
                          STT("dve", acc[:], us3[:, h, 0:128], sm[:, 16 + h:17 + h], acc[:], ALU.mult, ALU.add, ["us", "sm4", "acc"], ["acc"])
                      MEMSET("dve", acc[:, 0:1], 3e9, ["acc"])
                      P.op("dve", lambda e: e.max(out=m8[:, 0:8], in_=acc[:]), reads=["acc"], writes=["m8a"])
                      P.op("dve", lambda e: e.match_replace(out=accw[:], in_to_replace=m8[:, 0:8], in_values=acc[:], imm_value=-3e38),
                           reads=["acc", "m8a"], writes=["accw"])
                      P.op("dve", lambda e: e.max(out=m8[:, 8:16], in_=accw[:]), reads=["accw"], writes=["m8b"])
                      TS("dve", accw[:], acc[:], m8[:, 15:16], None, ALU.is_lt, None, ["acc", "m8b"], ["accw"])
                      TS("dve", selb[:], accw[:], NEG, None, ALU.mult, None, ["accw"], ["selb"])
                      nblk = 4 * j + 4
                      for c_ in range(j // 2 + 1):
                          nb_ = min(8, nblk - 8 * c_)
                          CP("dve" if c_ % 3 != 2 else "pool", v3(selx[:, c_ * 512:c_ * 512 + nb_ * 64], nb_), bc_last(selb[:, 8 * c_:8 * c_ + nb_], 64),
                             ["selb"], [("gT", c_)])
                      TT("dve", selx[:, 2 * j * 128:(2 * j + 2) * 128], selx[:, 2 * j * 128:(2 * j + 2) * 128], cbf["cmT"][:], ALU.add,
                         [("gT", j // 2), "c_cmT"], [("gT", j // 2)])
                      if dbg and j == 1 and g == 0:
                          DBG(acc[:], ["acc"], 128)
                      wk = [kt for kt in range(2 * j - 4, 2 * j + 2) if kt >= 0]
                      items = []
                      for kt in wk:
                          w_ = kt - (2 * j - 4)
                          mi = {0: 0, 1: 1, 4: 2, 5: 3}.get(w_)
                          maskT, mk = (None, None) if mi is None else (cbf["wmT"][:, mi * 128:(mi + 1) * 128], "c_wmT")
                          items.append((Kwin[:, (kt % 8) * 128:(kt % 8) * 128 + 128], "Kwin", maskT, mk,
                                        Vwin[:, (kt % 8) * 132 + g * 66:(kt % 8) * 132 + g * 66 + 65], "Vwin", kt))
                      run_branch(items)
                      combine(2)
                      nk = 2 * j + 2
                      items = [(Ksel[:, kt * 128:(kt + 1) * 128], "Ksel", selx[:, kt * 128:(kt + 1) * 128], ("gT", kt // 4),
                                Vsel[:, kt * 132 + g * 66:kt * 132 + g * 66 + 65], "Vsel", kt) for kt in range(nk)]
                      run_branch(items)
                      combine(1)
                  if dbg and j == 1:
                      DBG(onsa[:], ["onsa"], 512)
                  B = nxt()
                  TR4(mixr, "mixr", B)
                  CP("act", mixTp[:, 512:1024], ps[B][:], [pk(B)], ["mixTp"])
                  CP("act", onsab[:], onsa[:], ["onsa"], ["onsab"])
                  B = nxt()
                  TR4(onsab, "onsab", B)
                  CP("act", mixTp[:, 0:512], ps[B][:], [pk(B)], ["mixTp"])
                  STOP(7)
                  for q4 in range(2):
                      Y = nxt()
                      for o4 in range(4):
                          oc = q4 * 4 + o4
                          s = load_w("woutr", (oc,), 1024)
                          slab = v3(wbuf[s][:, 0:1024], 8)
                          for kc in range(8):
                              MM(ps[Y][:, o4 * 128:(o4 + 1) * 128], slab[:, kc, :], mixTp[:, kc * 128:(kc + 1) * 128], kc == 0, kc == 7,
                                 [("wbuf", s), "mixTp"], [pk(Y)])
                      zv = v3(xo[:], 8)[:, q4 * 4:q4 * 4 + 4, oc0:oc0 + 128]
                      STT("dve", zv, v3(ps[Y][:], 4), 1.0 / ALPHA, zv, ALU.mult, ALU.add, [pk(Y), "xo"], ["xo"])
              STOP(8)
              if m % 2 == 1:
                  st = m // 2
                  LN(xo, xob, 1, "xo", "xob")
                  if dbg and st == 0:
                      DBG(xo[:], ["xo"], 4096)
                  FFN(1, xo, xob, "xo", "xob")
                  LN(xo, xob, 2, "xo", "xob")
                  DMA(v3(pTb[:], 2), pT_d[:, :, st * 512:(st + 1) * 512].rearrange("c p t -> p c t"), [], ["pTb"], queue="pool")
                  for oc in range(8):
                      s = load_w("wgater", (oc,), 1024)
                      slab = v3(wbuf[s][:, 0:1024], 8)
                      G_ = nxt()
                      for kc in range(8):
                          MM(ps[G_][:], slab[:, kc, :], xob[:, kc * 512:(kc + 1) * 512], kc == 0, kc == 7, [("wbuf", s), "xob"], [pk(G_)])
                      a = nscr()
                      ACT(scr[a][:], ps[G_][:], AF.Sigmoid, [pk(G_)], [("scr", a)])
                      s2 = load_w("wpler", (oc,), 256)
                      slab2 = v3(wbuf[s2][:, 0:256], 2)
                      E_ = nxt()
                      for kc in range(2):
                          MM(ps[E_][:], slab2[:, kc, :], pTb[:, kc * 512:(kc + 1) * 512], kc == 0, kc == 1, [("wbuf", s2), "pTb"], [pk(E_)])
                      TT("dve", scr[a][:], scr[a][:], ps[E_][:], ALU.mult, [("scr", a), pk(E_)], [("scr", a)])
                      zc = xo[:, oc * 512:(oc + 1) * 512]
                      STT("dve", zc, scr[a][:], 1.0 / ALPHA, zc, ALU.mult, ALU.add, [("scr", a), "xo"], ["xo"])
                  LN(xo, xob, 3, "xo", "xob")
                  DMA(out_d[:, :, st * 512:(st + 1) * 512].rearrange("c p t -> p c t"), v3(xo[:], 8), ["xo"], [("out", st)], queue="pool")

        try:
            main_loop()
        except _Stop:
            pass
        outs = [("out", st) for st in range(NB // 2)] + ([("dbg", s) for s in range(dbg_n[0])] if dbg else [])
        P.op("sp", None, reads=outs)
        P.emit(sems)
    return nc, P


_NC_CACHE = {}


def make_in_maps(inp, T):
    x = np.asarray(inp["x"], np.float32)
    p = np.asarray(inp["p"], np.float32)[0]
    B = x.shape[0]
    w = layout_weights({k: np.asarray(v, np.float32) for k, v in inp.items()})
    maps = []
    for core in range(2 * B):
        b, par = core // 2, core % 2
        d = dict(w)
        d.update(make_consts(T, par))
        d["xT"] = np.ascontiguousarray(x[b].T).reshape(8, 128, T)
        own = p[b].reshape(T // 256, 2, 128, 256)[:, par].reshape(T // 2, 256)
        d["pT"] = np.ascontiguousarray(own.T).reshape(2, 128, T // 2)
        maps.append(d)
    return maps


def assemble(results, B, T):
    out = np.zeros((B, T, D), np.float32)
    for core in range(2 * B):
        b, par = core // 2, core % 2
        o = results[core]["outT"].reshape(D, T // 2).T
        out[b].reshape(T // 256, 2, 128, D)[:, par] = o.reshape(T // 256, 128, D)
    return out


def kernel(**inputs):
    x = inputs["x"]
    B, T = x.shape[0], x.shape[1]
    key = T
    if key not in _NC_CACHE:
        _NC_CACHE[key] = build_nc(T)[0]
    nc = _NC_CACHE[key]
    maps = make_in_maps(inputs, T)
    res = run_bass_kernel_spmd(nc, maps, core_ids=list(range(2 * B)))
    return assemble(res.results, B, T)
```
